# Optimizing a Trainium2 kernel written in Bass

```python
import jax, jax.numpy as jnp
from jax import lax
import numpy as np

D_MODEL = 1024
BATCH = 32
SEQ = 2048
DEPTH = 4

MEM_LEN = 256
D_CONV = D_MODEL
CONV_KERNEL = 31
D_SHORT = D_MODEL
SHORT_KERNEL = 3
D_POOL = D_MODEL
POOL_WINDOWS = (2, 4, 8, 16)
N_POOL_GROUPS = 4
POOL_GROUP = D_POOL // N_POOL_GROUPS
N_BRANCHES = 3
D_IN_PROJ = 2 * D_CONV + 3 * D_SHORT + D_POOL + N_BRANCHES * D_MODEL
N_XHEADS = 4
XHEAD_DIM = D_MODEL // N_XHEADS
N_EXPERTS = 32
TOP_K = 4
D_EXPERT = D_MODEL
SWIGLU_LIMIT = 7.0
SWIGLU_ALPHA = 1.702
EXPERT_BLOCK = 128
DEEPNORM_ALPHA = float((2 * DEPTH) ** 0.25)
DEEPNORM_BETA = float((8 * DEPTH) ** -0.25)
LN_EPS = 1e-5

kernel_name = "hybrid_conv_pool_xattn_moe_deepnorm"


def layer_norm(x, g, b):
    xf = x.astype(jnp.float32)
    mu = jnp.mean(xf, axis=-1, keepdims=True)
    var = jnp.mean(jnp.square(xf - mu), axis=-1, keepdims=True)
    y = (xf - mu) * lax.rsqrt(var + LN_EPS)
    return (y * g + b).astype(x.dtype)


def causal_depthwise_conv(u, w):
    k_width, ch = w.shape
    return lax.conv_general_dilated(
        u, w[:, None, :].astype(u.dtype), window_strides=(1,), padding=[(k_width - 1, 0)],
        dimension_numbers=("NWC", "WIO", "NWC"), feature_group_count=ch)


def causal_multiscale_pool(u):
    bsz, t_len, _ = u.shape
    uf = u.astype(jnp.float32).reshape(bsz, t_len, N_POOL_GROUPS, POOL_GROUP)
    cs = jnp.cumsum(uf, axis=1)
    pos = jnp.arange(t_len)
    outs = []
    for gi, win in enumerate(POOL_WINDOWS):
        c = cs[:, :, gi]
        prev = jnp.pad(c, ((0, 0), (win, 0), (0, 0)))[:, :t_len]
        cnt = jnp.minimum(pos + 1, win).astype(jnp.float32)
        outs.append((c - prev) / cnt[None, :, None])
    pooled = jnp.stack(outs, axis=2)
    return (pooled - uf).astype(u.dtype)


def mixer_sublayer(x, w_in, b_in, conv_a_w, conv_a_b, ln_a_g, ln_a_b, w_a_out, b_a_out,
                   conv_b_w, w_b_out, pool_w, pool_scale, w_mix_out):
    bsz, t_len, _ = x.shape
    p = x @ w_in + b_in
    o1 = 2 * D_CONV
    o2 = o1 + D_SHORT
    o3 = o2 + D_SHORT
    o4 = o3 + D_SHORT
    o5 = o4 + D_POOL
    a_in, gate_b, gate_c, v_bc, pool_in, gates = (
        p[..., :o1], p[..., o1:o2], p[..., o2:o3], p[..., o3:o4], p[..., o4:o5], p[..., o5:])
    a = a_in[..., :D_CONV] * jax.nn.sigmoid(a_in[..., D_CONV:])
    a = causal_depthwise_conv(a, conv_a_w) + conv_a_b
    a = jax.nn.silu(layer_norm(a, ln_a_g, ln_a_b))
    y_a = a @ w_a_out + b_a_out
    u = causal_depthwise_conv(gate_c * v_bc, conv_b_w)
    y_b = (gate_b * u) @ w_b_out
    pooled = causal_multiscale_pool(pool_in)
    y_c = jnp.einsum("btgc,gcd->btgd", pooled, pool_w).reshape(bsz, t_len, D_POOL) * pool_scale
    g_a = gates[..., :D_MODEL]
    g_b = gates[..., D_MODEL:2 * D_MODEL]
    g_c = gates[..., 2 * D_MODEL:]
    m = jax.nn.sigmoid(g_a) * y_a + jax.nn.sigmoid(g_b) * y_b + jax.nn.sigmoid(g_c) * y_c
    return m @ w_mix_out


def memory_cross_attention(x, mem_n, w_q, w_k, w_v, w_o):
    bsz, t_len, d = x.shape
    m_len = mem_n.shape[1]
    q = (x @ w_q).reshape(bsz, t_len, N_XHEADS, XHEAD_DIM)
    k = (mem_n @ w_k).reshape(bsz, m_len, N_XHEADS, XHEAD_DIM)
    v = (mem_n @ w_v).reshape(bsz, m_len, N_XHEADS, XHEAD_DIM)
    s = jnp.einsum("bshk,bmhk->bhsm", q.astype(jnp.float32), k.astype(jnp.float32)) * (XHEAD_DIM ** -0.5)
    prob = jax.nn.softmax(s, axis=-1).astype(v.dtype)
    o = jnp.einsum("bhsm,bmhk->bshk", prob, v).reshape(bsz, t_len, d)
    return o @ w_o


def clamped_swiglu(gu):
    gate = jnp.minimum(gu[..., :D_EXPERT], SWIGLU_LIMIT)
    up = jnp.clip(gu[..., D_EXPERT:], -SWIGLU_LIMIT, SWIGLU_LIMIT)
    return (up + 1.0) * (gate * jax.nn.sigmoid(gate * SWIGLU_ALPHA))


def moe_sublayer(h, router_w, router_b, w_gu, b_gu, w_down, b_down):
    bsz, t_len, d = h.shape
    n_tok = bsz * t_len
    hf = h.reshape(n_tok, d)
    logits = (hf @ router_w).astype(jnp.float32) + router_b.astype(jnp.float32)
    top_logits, top_idx = lax.top_k(logits, TOP_K)
    gates = jax.nn.softmax(top_logits, axis=-1)
    n_assign = n_tok * TOP_K
    flat_e = top_idx.reshape(n_assign).astype(jnp.int32)
    flat_tok = jnp.arange(n_assign, dtype=jnp.int32) // TOP_K
    flat_gate = gates.reshape(n_assign)
    order = jnp.argsort(flat_e)
    e_sorted = flat_e[order]
    counts = jnp.bincount(flat_e, length=N_EXPERTS).astype(jnp.int32)
    padded = ((counts + EXPERT_BLOCK - 1) // EXPERT_BLOCK) * EXPERT_BLOCK
    pad_end = jnp.cumsum(padded)
    pad_start = pad_end - padded
    grp_start = jnp.cumsum(counts) - counts
    dest = pad_start[e_sorted] + (jnp.arange(n_assign, dtype=jnp.int32) - grp_start[e_sorted])
    n_blocks = (n_assign + N_EXPERTS * (EXPERT_BLOCK - 1) + EXPERT_BLOCK - 1) // EXPERT_BLOCK
    n_rows = n_blocks * EXPERT_BLOCK
    row_tok = jnp.full((n_rows,), n_tok, jnp.int32).at[dest].set(flat_tok[order])
    row_gate = jnp.zeros((n_rows,), jnp.float32).at[dest].set(flat_gate[order])
    block_start = jnp.arange(n_blocks, dtype=jnp.int32) * EXPERT_BLOCK
    block_e = jnp.minimum(jnp.searchsorted(pad_end, block_start, side="right"), N_EXPERTS - 1)
    h_pad = jnp.concatenate([hf, jnp.zeros((1, d), hf.dtype)], axis=0)

    def step(acc, blk):
        tok, g, e = blk
        xb = h_pad[tok]
        act = clamped_swiglu(xb @ w_gu[e] + b_gu[e])
        yb = act @ w_down[e] + b_down[e]
        return acc.at[tok].add(yb * g[:, None].astype(yb.dtype)), None

    acc0 = jnp.zeros((n_tok + 1, d), h.dtype)
    acc, _ = lax.scan(step, acc0, (row_tok.reshape(n_blocks, EXPERT_BLOCK),
                                   row_gate.reshape(n_blocks, EXPERT_BLOCK), block_e))
    return acc[:n_tok].reshape(bsz, t_len, d)


def setup_inputs(seed: int = 0) -> dict:
    key = jax.random.key(seed)
    ks = jax.random.split(key, 40)
    L = DEPTH
    f32 = jnp.float32

    def nrm(k, shape, scale):
        return jax.random.normal(k, shape, f32) * scale

    def gain(k, shape):
        return 1.0 + 0.05 * jax.random.normal(k, shape, f32)

    def bias(k, shape, scale=0.02):
        return scale * jax.random.normal(k, shape, f32)

    return {
        "x": nrm(ks[0], (BATCH, SEQ, D_MODEL), 1.0),
        "mem": nrm(ks[1], (BATCH, MEM_LEN, D_MODEL), 1.0),
        "mem_ln_g": gain(ks[2], (D_MODEL,)),
        "mem_ln_b": bias(ks[3], (D_MODEL,)),
        "w_in": nrm(ks[4], (L, D_MODEL, D_IN_PROJ), D_MODEL ** -0.5),
        "b_in": bias(ks[5], (L, D_IN_PROJ)),
        "conv_a_w": nrm(ks[6], (L, CONV_KERNEL, D_CONV), CONV_KERNEL ** -0.5),
        "conv_a_b": bias(ks[7], (L, D_CONV)),
        "ln_a_g": gain(ks[8], (L, D_CONV)),
        "ln_a_b": bias(ks[9], (L, D_CONV)),
        "w_a_out": nrm(ks[10], (L, D_CONV, D_MODEL), D_CONV ** -0.5),
        "b_a_out": bias(ks[11], (L, D_MODEL)),
        "conv_b_w": nrm(ks[12], (L, SHORT_KERNEL, D_SHORT), SHORT_KERNEL ** -0.5),
        "w_b_out": nrm(ks[13], (L, D_SHORT, D_MODEL), D_SHORT ** -0.5),
        "pool_w": nrm(ks[14], (L, N_POOL_GROUPS, POOL_GROUP, POOL_GROUP), POOL_GROUP ** -0.5),
        "pool_scale": gain(ks[15], (L, D_POOL)),
        "w_mix_out": nrm(ks[16], (L, D_MODEL, D_MODEL), DEEPNORM_BETA * D_MODEL ** -0.5),
        "ln1_g": gain(ks[17], (L, D_MODEL)),
        "ln1_b": bias(ks[18], (L, D_MODEL)),
        "w_xq": nrm(ks[19], (L, D_MODEL, D_MODEL), D_MODEL ** -0.5),
        "w_xk": nrm(ks[20], (L, D_MODEL, D_MODEL), D_MODEL ** -0.5),
        "w_xv": nrm(ks[21], (L, D_MODEL, D_MODEL), DEEPNORM_BETA * D_MODEL ** -0.5),
        "w_xo": nrm(ks[22], (L, D_MODEL, D_MODEL), DEEPNORM_BETA * D_MODEL ** -0.5),
        "ln2_g": gain(ks[23], (L, D_MODEL)),
        "ln2_b": bias(ks[24], (L, D_MODEL)),
        "router_w": nrm(ks[25], (L, D_MODEL, N_EXPERTS), D_MODEL ** -0.5),
        "router_b": bias(ks[26], (L, N_EXPERTS), 0.01),
        "w_gu": nrm(ks[27], (L, N_EXPERTS, D_MODEL, 2 * D_EXPERT), D_MODEL ** -0.5),
        "b_gu": bias(ks[28], (L, N_EXPERTS, 2 * D_EXPERT), 0.01),
        "w_down": nrm(ks[29], (L, N_EXPERTS, D_EXPERT, D_MODEL), DEEPNORM_BETA * D_EXPERT ** -0.5),
        "b_down": bias(ks[30], (L, N_EXPERTS, D_MODEL), 0.01),
        "ln3_g": gain(ks[31], (L, D_MODEL)),
        "ln3_b": bias(ks[32], (L, D_MODEL)),
    }


def reference(x, mem, mem_ln_g, mem_ln_b, w_in, b_in, conv_a_w, conv_a_b, ln_a_g, ln_a_b,
              w_a_out, b_a_out, conv_b_w, w_b_out, pool_w, pool_scale, w_mix_out, ln1_g, ln1_b,
              w_xq, w_xk, w_xv, w_xo, ln2_g, ln2_b, router_w, router_b, w_gu, b_gu, w_down,
              b_down, ln3_g, ln3_b):
    mem_n = layer_norm(mem, mem_ln_g, mem_ln_b)
    for l in range(DEPTH):
        mix = mixer_sublayer(x, w_in[l], b_in[l], conv_a_w[l], conv_a_b[l], ln_a_g[l], ln_a_b[l],
                             w_a_out[l], b_a_out[l], conv_b_w[l], w_b_out[l], pool_w[l],
                             pool_scale[l], w_mix_out[l])
        x = layer_norm(DEEPNORM_ALPHA * x + mix, ln1_g[l], ln1_b[l])
        xa = memory_cross_attention(x, mem_n, w_xq[l], w_xk[l], w_xv[l], w_xo[l])
        x = layer_norm(DEEPNORM_ALPHA * x + xa, ln2_g[l], ln2_b[l])
        ff = moe_sublayer(x, router_w[l], router_b[l], w_gu[l], b_gu[l], w_down[l], b_down[l])
        x = layer_norm(DEEPNORM_ALPHA * x + ff, ln3_g[l], ln3_b[l])
    return x
```

```python
import numpy as np
import concourse.bass as bass
import concourse.mybir as mybir
from concourse.bass_utils import run_bass_kernel_spmd

F32 = mybir.dt.float32
BF16 = mybir.dt.bfloat16
AF = mybir.ActivationFunctionType
ALU = mybir.AluOpType
AX = mybir.AxisListType

D = 1024
T = 2048
MEM = 256
NE = 32
FIN = 9216
DEPTH = 4
ALPHA = float((2 * DEPTH) ** 0.25)
EPS = 1e-5
NCORES = 8
NSEQ = 4

O_BIN, O_CAW, O_CAB, O_LAG, O_LAB, O_BAO, O_CBW, O_PSC, O_BGU = 0, 72, 320, 328, 336, 344, 352, 376, 384
NPF = 896

EPOCH = 24000
ENGS = ("pe", "act", "dve", "pool", "sp")


class Buf:
    __slots__ = ("name", "writer", "readers", "dma_readers", "sem", "ndma")

    def __init__(self, name, writer=None):
        self.name = name
        self.writer = writer
        self.readers = {}
        self.dma_readers = []
        self.sem = None
        self.ndma = 0


class Op:
    __slots__ = ("eng", "fn", "deps", "is_dma", "buf", "dma_val", "needs_inc", "ev", "waits", "seq")

    def __init__(self, eng, fn):
        self.eng = eng
        self.fn = fn
        self.deps = []
        self.is_dma = False
        self.buf = None
        self.dma_val = 0
        self.needs_inc = False
        self.ev = None
        self.waits = None
        self.seq = 0


class Prog:
    def __init__(self, nc):
        self.nc = nc
        self.streams = {e: [] for e in ENGS}
        self.nops = 0
        self.bufs = []
        self.scratch_live = []
        self.barrier_op = None

    def buf(self, name, scratch=False):
        b = Buf(name, self.barrier_op if scratch else None)
        self.bufs.append(b)
        if scratch:
            self.scratch_live.append(b)
        return b

    def op(self, eng, fn, R=(), W=(), dma=None, ndma=1):
        o = Op(eng, fn)
        self.nops += 1
        o.seq = self.nops
        deps = {}

        def add(d):
            if d is None or d is o:
                return
            if d.is_dma:
                deps[id(d)] = d
                return
            if d.eng == "pe" and eng == "pe":
                return
            k = d.eng
            if k not in deps or deps[k].seq < d.seq:
                deps[k] = d

        for b in R:
            add(b.writer)
        for b in W:
            add(b.writer)
            for r in b.readers.values():
                add(r)
            for r in b.dma_readers:
                add(r)
        o.deps = list(deps.values())
        for d in o.deps:
            if not d.is_dma:
                d.needs_inc = True
        if dma is not None:
            o.is_dma = True
            o.buf = dma
            dma.ndma += ndma
            o.dma_val = 16 * dma.ndma
        for b in W:
            b.writer = o
            b.readers = {}
            b.dma_readers = []
        for b in R:
            if b.writer is o:
                continue
            if o.is_dma:
                b.dma_readers.append(o)
            else:
                b.readers[eng] = o
        self.streams[eng].append(o)
        return o

    def phase_barrier(self, dummy_ap):
        live = self.scratch_live
        self.scratch_live = []
        tok = Buf("phase_tok")
        o = self.op("dve", lambda e: e.memset(dummy_ap, 0.0), R=(), W=live + [tok])
        o.needs_inc = True
        self.barrier_op = o
        return o

    def emit(self, final_bufs):
        nc = self.nc
        sems = {}

        def get_sem(key):
            if key not in sems:
                sems[key] = nc.alloc_semaphore("s_%s_%d" % key)
            return sems[key]

        for e in ENGS:
            cnt = 0
            for o in self.streams[e]:
                if o.is_dma:
                    if o.buf.sem is None:
                        o.buf.sem = nc.alloc_semaphore("d%d_%s" % (len(sems) + o.seq, o.buf.name))
                    continue
                if o.needs_inc:
                    ep, v = divmod(cnt, EPOCH)
                    o.ev = (e, ep, v + 1)
                    cnt += 1
        for e in ENGS:
            seen_c = {}
            seen_d = {}
            for o in self.streams[e]:
                w = []
                for d in o.deps:
                    if d.is_dma:
                        s = d.buf.sem
                        if seen_d.get(id(s), 0) < d.dma_val:
                            seen_d[id(s)] = d.dma_val
                            w.append((s, d.dma_val))
                    else:
                        pe, ep, v = d.ev
                        if seen_c.get(pe, (-1, 0)) < (ep, v):
                            seen_c[pe] = (ep, v)
                            w.append((get_sem((pe, ep)), v))
                o.waits = w
        engobj = {"pe": nc.tensor, "act": nc.scalar, "dve": nc.vector, "pool": nc.gpsimd, "sp": nc.sync}
        for e in ENGS:
            eng = engobj[e]
            for o in self.streams[e]:
                for (s, v) in o.waits:
                    eng.wait_ge(s, v)
                r = o.fn(eng)
                if o.is_dma:
                    if not isinstance(r, (list, tuple)):
                        r = [r]
                    for ins in r:
                        ins.then_inc(o.buf.sem, 16)
                elif o.needs_inc:
                    pe, ep, v = o.ev
                    r.then_inc(get_sem((pe, ep)), 1)
        dma_sems = [b.sem for b in self.bufs if b.sem is not None]
        for b in self.bufs:
            if b.sem is not None:
                nc.sync.wait_ge(b.sem, 16 * b.ndma)
        return list(sems.values()) + dma_sems


class Builder:
    def __init__(self, L, NS, do_mixer=True, do_attn=True, do_moe=True, ne=NE):
        self.L, self.NS = L, NS
        self.do_mixer, self.do_attn, self.do_moe, self.ne = do_mixer, do_attn, do_moe, ne
        nc = self.nc = bass.Bass("TRN2", target_bir_lowering=False)
        P = self.P = Prog(nc)

        def din(name, shape):
            return nc.dram_tensor(name, list(shape), F32, kind="ExternalInput").ap()

        self.x = din("x", [NS, T, D])
        self.mem = din("mem", [NS, MEM, D])
        self.memln = din("memln", [2, D])
        self.ident_d = din("ident", [128, 128])
        self.pfm = din("pfm", [L, 128, NPF])
        self.rows = din("rows", [L, 6, D])
        self.router_b = din("router_b", [L, NE])
        self.w_in = din("w_in", [L, D, FIN])
        self.w_a_out = din("w_a_out", [L, D, D])
        self.w_b_out = din("w_b_out", [L, D, D])
        self.pool_w = din("pool_w", [L, 4, 256, 256])
        self.w_mix = din("w_mix_out", [L, D, D])
        self.w_xq = din("w_xq", [L, D, D])
        self.w_xk = din("w_xk", [L, D, D])
        self.w_xv = din("w_xv", [L, D, D])
        self.w_xo = din("w_xo", [L, D, D])
        self.router_w = din("router_w", [L, D, NE])
        self.w_gu = din("w_gu", [L, NE, D, 2 * D])
        self.w_down = din("w_down", [L, NE, D, D])
        self.b_down = din("b_down", [L, NE, D])
        self.y = nc.dram_tensor("y", [NS, T, D], F32, kind="ExternalOutput").ap()

        def sb(name, shape, dt):
            return nc.alloc_sbuf_tensor(name, list(shape), dt)

        self.X = sb("X", [128, 16, D], F32)
        self.bX = [P.buf("X%d" % i) for i in range(16)]
        self.XT = sb("XT", [128, 8, T], BF16)
        self.bXT = [P.buf("XT%d" % i) for i in range(16)]
        self.MEMT = sb("MEMT", [128, 8, MEM], BF16)
        self.bMEMT = [P.buf("MEMT%d" % i) for i in range(2)]
        self.GB = sb("GB", [128, 2, D], F32)
        self.bGB = P.buf("GB")
        self.XB = [sb("XB%d" % i, [128, D], BF16) for i in range(2)]
        self.bXB = [P.buf("XB%d" % i) for i in range(2)]
        self.xb_i = 0
        self.IDF = sb("IDF", [128, 128], F32)
        self.IDENT = sb("IDENT", [128, 128], BF16)
        self.bID = P.buf("ident")
        self.ONES = sb("ONES", [128, 128], BF16)
        self.bONES = P.buf("ones")
        self.RC15 = sb("RC15", [128, 16], F32)
        self.bRC = P.buf("rc15")
        self.EPSC = sb("EPSC", [128, 1], F32)
        self.PFM = sb("PFM", [128, NPF], F32)
        self.bPFM = P.buf("pfm")
        self.PW = sb("PW", [128, 4, 2, 256], BF16)
        self.bPW = P.buf("pw")
        self.RW = sb("RW", [128, 8, NE], BF16)
        self.bRW = P.buf("rw")
        self.RB = sb("RB", [128, NE], F32)
        self.bRB = P.buf("rb")
        self.BD = sb("BD", [NE, D], BF16)
        self.bBD = P.buf("bd")
        self.HA = sb("HA", [128, 8, 30], F32)
        self.HBv = sb("HBv", [128, 8, 2], F32)
        self.HC = sb("HC", [128, 8, 16], F32)
        self.bHA = [P.buf("HA%d" % c) for c in range(8)]
        self.bHB = [P.buf("HB%d" % c) for c in range(8)]
        self.bHC = [P.buf("HC%d" % c) for c in range(8)]
        self.DUMMY = sb("DUMMY", [128, 8], F32)
        self.NST = 4
        self.BNS = [sb("BNS%d" % i, [128, 2, 6], F32) for i in range(self.NST)]
        self.MV = [sb("MV%d" % i, [128, 4], F32) for i in range(self.NST)]
        self.bST = [P.buf("ST%d" % i) for i in range(self.NST)]
        self.st_i = 0
        self.NWS = 6
        self.WS = [sb("WS%d" % i, [128, 8, 128], BF16) for i in range(self.NWS)]
        self.bWS = [P.buf("WS%d" % i) for i in range(self.NWS)]
        self.ws_i = 0
        self.NWL = 2
        self.WL = [sb("WL%d" % i, [128, 8, 512], BF16) for i in range(self.NWL)]
        self.bWL = [P.buf("WL%d" % i) for i in range(self.NWL)]
        self.wl_i = 0
        self.PS = [nc.alloc_psum_tensor("ps%d" % i, [128, 512], F32) for i in range(8)]
        self.bPS = [P.buf("ps%d" % i) for i in range(8)]
        self.ps_i = 0
        self.ps_n = 8
        self.SCR_BYTES = (nc.sbuf_bytes_remaining - 1024) // 64 * 64
        assert self.SCR_BYTES >= 52 * 1024, self.SCR_BYTES
        self.SCR = sb("SCR", [128, self.SCR_BYTES // 2], BF16)
        self.scr_off = 0

    def carve(self, name, free_shape, dt, nbuf=1):
        n = int(np.prod(free_shape))
        nbytes = n * (4 if dt == F32 else 2)
        nbytes_al = (nbytes + 31) // 32 * 32
        assert self.scr_off + nbytes_al <= self.SCR_BYTES, (name, self.scr_off, nbytes_al)
        ap = self.SCR[:, self.scr_off // 2:(self.scr_off + nbytes) // 2]
        self.scr_off += nbytes_al
        if dt == F32:
            ap = ap.bitcast(F32)
        if len(free_shape) == 2:
            ap = ap.rearrange("p (a b) -> p a b", a=free_shape[0])
        elif len(free_shape) == 3:
            ap = ap.rearrange("p (a b c) -> p a b c", a=free_shape[0], b=free_shape[1])
        bufs = [self.P.buf(name + str(i), scratch=True) for i in range(nbuf)]
        return ap, (bufs[0] if nbuf == 1 else bufs)

    def new_phase(self):
        self.P.phase_barrier(self.DUMMY[:, 0:1])
        self.scr_off = 0

    def ps_next(self):
        i = self.ps_i % self.ps_n
        self.ps_i += 1
        return self.PS[i], self.bPS[i]

    def wcols(self, w2d, c0, n):
        return w2d.rearrange("(kc p) f -> p kc f", p=128)[:, :, c0:c0 + n]

    def load_ws(self, src):
        s = self.ws_i % self.NWS
        self.ws_i += 1
        W, b = self.WS[s], self.bWS[s]
        self.P.op("pool", lambda e: e.dma_start(out=W[:], in_=src), W=[b], dma=b)
        return W, b

    def load_wl(self, src):
        s = self.wl_i % self.NWL
        self.wl_i += 1
        W, b = self.WL[s], self.bWL[s]
        self.P.op("pool", lambda e: e.dma_start(out=W[:], in_=src), W=[b], dma=b)
        return W, b

    def mm(self, out_ap, pairs, R, bps):
        pairs = list(pairs)

        def fn(e):
            n = len(pairs)
            for i, (l, r) in enumerate(pairs):
                ins = e.matmul(out_ap, l, r, start=(i == 0), stop=(i == n - 1))
            return ins

        self.P.op("pe", fn, R=R, W=[bps])

    def dve(self, fn, R, W):
        return self.P.op("dve", fn, R=R, W=W)

    def act(self, fn, R, W):
        return self.P.op("act", fn, R=R, W=W)

    def setup_consts(self):
        P = self.P
        IDF, IDENT, ONES, RC = self.IDF, self.IDENT, self.ONES, self.RC15
        P.op("sp", lambda e: e.dma_start(out=IDF[:], in_=self.ident_d), W=[self.bID], dma=self.bID)
        self.dve(lambda e: e.tensor_copy(IDENT[:], IDF[:]), R=[self.bID], W=[self.bID])
        self.dve(lambda e: e.memset(ONES[:], 1.0), R=[], W=[self.bONES])
        for t in range(16):
            self.dve(lambda e, t=t: e.memset(RC[:, t:t + 1], 1.0 / (t + 1)), R=[], W=[self.bRC])
        self.dve(lambda e: e.memset(self.EPSC[:], EPS), R=[], W=[self.bRC])

    def load_layer_params(self, l):
        P = self.P
        P.op("sp", lambda e: e.dma_start(out=self.PFM[:], in_=self.pfm[l]), W=[self.bPFM], dma=self.bPFM)
        P.op("sp", lambda e: e.dma_start(out=self.RB[:], in_=self.router_b[l].partition_broadcast(128)),
             W=[self.bRB], dma=self.bRB)
        pw_src = self.pool_w[l].rearrange("g (cc p) d -> p g cc d", p=128)

        def ld_pw(e):
            return [e.dma_start(out=self.PW[:, g], in_=pw_src[:, g]) for g in range(4)]

        P.op("pool", ld_pw, W=[self.bPW], dma=self.bPW, ndma=4)
        P.op("pool", lambda e: e.dma_start(out=self.RW[:], in_=self.wcols(self.router_w[l], 0, NE)),
             W=[self.bRW], dma=self.bRW)
        P.op("pool", lambda e: e.dma_start(out=self.BD[:], in_=self.b_down[l]), W=[self.bBD], dma=self.bBD)

    def load_gb(self, g_row, b_row):
        def f(e):
            return [e.dma_start(out=self.GB[:, 0, :], in_=g_row.partition_broadcast(128)),
                    e.dma_start(out=self.GB[:, 1, :], in_=b_row.partition_broadcast(128))]

        self.P.op("sp", f, W=[self.bGB], dma=self.bGB, ndma=2)

    def to_T(self, row_ap, brow, dstT, bdst):
        s = self.xb_i % 2
        self.xb_i += 1
        XB, bXB = self.XB[s], self.bXB[s]
        self.act(lambda e: e.activation(out=XB[:], in_=row_ap, func=AF.Copy), R=[brow], W=[bXB])
        ps, bps = self.ps_next()
        psb = ps[:].bitcast(BF16)

        def tr(e):
            for c in range(8):
                ins = e.transpose(psb[:, c * 128:(c + 1) * 128], XB[:, c * 128:(c + 1) * 128], self.IDENT[:])
            return ins

        self.P.op("pe", tr, R=[bXB, self.bID], W=[bps])
        self.act(lambda e: e.activation(out=dstT, in_=psb.rearrange("p (c t) -> p c t", c=8), func=AF.Copy),
                 R=[bps], W=[bdst])

    def ln_rows(self, row_ap, brow, dstT, bdst):
        k = self.st_i % self.NST
        self.st_i += 1
        BNS, MV, bST = self.BNS[k], self.MV[k], self.bST[k]
        self.dve(lambda e: e.bn_stats(BNS[:, 0, :], row_ap[:, 0:512]), R=[brow], W=[bST])
        self.dve(lambda e: e.bn_stats(BNS[:, 1, :], row_ap[:, 512:1024]), R=[brow], W=[bST])
        self.dve(lambda e: e.bn_aggr(MV[:, 0:2], BNS[:].rearrange("p a b -> p (a b)")), R=[bST], W=[bST])
        self.act(lambda e: e.activation(out=MV[:, 2:3], in_=MV[:, 1:2], func=AF.Sqrt, bias=self.EPSC[:, 0:1], scale=1.0),
                 R=[bST, self.bRC], W=[bST])
        self.dve(lambda e: e.reciprocal(MV[:, 2:3], MV[:, 2:3]), R=[bST], W=[bST])
        self.dve(lambda e: e.scalar_tensor_tensor(MV[:, 3:4], MV[:, 0:1], -1.0, MV[:, 2:3], ALU.mult, ALU.mult),
                 R=[bST], W=[bST])
        self.act(lambda e: e.activation(out=row_ap, in_=row_ap, func=AF.Identity, bias=MV[:, 3:4], scale=MV[:, 2:3]),
                 R=[brow, bST], W=[brow])
        self.dve(lambda e: e.tensor_tensor(row_ap, row_ap, self.GB[:, 0, :], ALU.mult), R=[brow, self.bGB], W=[brow])
        self.dve(lambda e: e.tensor_tensor(row_ap, row_ap, self.GB[:, 1, :], ALU.add), R=[brow, self.bGB], W=[brow])
        self.to_T(row_ap, brow, dstT, bdst)

    def ln_x(self, i):
        self.ln_rows(self.X[:, i, :], self.bX[i], self.XT[:, :, i * 128:(i + 1) * 128], self.bXT[i])

    def resid(self, i, half, ps_ap, bps):
        xs = self.X[:, i, half * 512:(half + 1) * 512]
        self.dve(lambda e: e.scalar_tensor_tensor(xs, xs, ALPHA, ps_ap, ALU.mult, ALU.add),
                 R=[self.bX[i], bps], W=[self.bX[i]])

    def load_seq(self, s):
        P = self.P
        for i in range(16):
            P.op("sp", lambda e, i=i: e.dma_start(out=self.X[:, i, :], in_=self.x[s, i * 128:(i + 1) * 128, :]),
                 W=[self.bX[i]], dma=self.bX[i])
        for i in range(16):
            self.to_T(self.X[:, i, :], self.bX[i], self.XT[:, :, i * 128:(i + 1) * 128], self.bXT[i])
        self.new_phase()
        MR, bMR = self.carve("MR", [2, D], F32, nbuf=2)
        self.load_gb(self.memln[0], self.memln[1])
        for mc in range(2):
            P.op("sp", lambda e, mc=mc: e.dma_start(out=MR[:, mc, :], in_=self.mem[s, mc * 128:(mc + 1) * 128, :]),
                 W=[bMR[mc]], dma=bMR[mc])
            self.ln_rows(MR[:, mc, :], bMR[mc], self.MEMT[:, :, mc * 128:(mc + 1) * 128], self.bMEMT[mc])

    def store_seq(self, s):
        for i in range(16):
            self.P.op("sp", lambda e, i=i: e.dma_start(out=self.y[s, i * 128:(i + 1) * 128, :], in_=self.X[:, i, :]),
                      R=[self.bX[i]], dma=self.bX[i])

    def mixer_tile(self, l, tt):
        P = self.P
        PFM = self.PFM
        w_in = self.w_in[l]
        xt_bufs = [self.bXT[4 * tt + s] for s in range(4)]
        tsl = slice(tt * 512, (tt + 1) * 512)

        def inproj(j):
            W, bW = self.load_ws(self.wcols(w_in, j * 128, 128))
            ps, bps = self.ps_next()
            self.mm(ps[:], [(W[:, k, :], self.XT[:, k, tsl]) for k in range(8)], R=[bW] + xt_bufs, bps=bps)
            return ps, bps

        def bias(off, j):
            return PFM[:, off + j:off + j + 1]

        self.new_phase()
        A, bA = self.carve("A", [8, 512], BF16, nbuf=8)
        YB, bYB = self.carve("YB", [8, 512], BF16, nbuf=8)
        PL, bPL = self.carve("PL", [8, 512], BF16, nbuf=8)
        GA, bGA = self.carve("GA", [512], F32)
        ABUF, bAB = self.carve("ABUF", [544], F32)
        ACC, bACC = self.carve("ACC", [512], F32)
        GC, bGC = self.carve("GC", [512], F32)
        GV, bGV = self.carve("GV", [516], F32)
        U, bU = self.carve("U", [512], F32)
        PIN, bPIN = self.carve("PIN", [528], F32)
        S0, bS0 = self.carve("S0", [528], F32)
        S1, bS1 = self.carve("S1", [528], F32)
        SQ, bSQ = self.carve("SQ", [2, 512], BF16, nbuf=2)
        MEANB, bMEANB = self.carve("MEANB", [512], F32)
        RSTDB, bRSTDB = self.carve("RSTDB", [512], F32)
        XH, bXH = self.carve("XH", [512], F32)
        self.ps_n = 6
        psS1, bpsS1 = self.PS[6], self.bPS[6]
        psS2, bpsS2 = self.PS[7], self.bPS[7]

        for c in range(8):
            ps_l, bps_l = inproj(c)
            ps_g, bps_g = inproj(8 + c)
            self.act(lambda e, ps_g=ps_g, c=c: e.activation(out=GA[:], in_=ps_g[:], func=AF.Sigmoid,
                                                            bias=bias(O_BIN, 8 + c), scale=1.0),
                     R=[bps_g, self.bPFM], W=[bGA])
            if tt == 0:
                self.dve(lambda e: e.memset(ABUF[:, 0:30], 0.0), R=[], W=[bAB])
            else:
                self.dve(lambda e, c=c: e.tensor_copy(ABUF[:, 0:30], self.HA[:, c, :]), R=[self.bHA[c]], W=[bAB])
            self.dve(lambda e, ps_l=ps_l, c=c: e.scalar_tensor_tensor(ABUF[:, 30:542], ps_l[:], bias(O_BIN, c), GA[:],
                                                                      ALU.add, ALU.mult),
                     R=[bps_l, bGA, self.bPFM], W=[bAB])
            if tt < 3:
                self.act(lambda e, c=c: e.activation(out=self.HA[:, c, :], in_=ABUF[:, 512:542], func=AF.Copy),
                         R=[bAB], W=[self.bHA[c]])
            self.dve(lambda e, c=c: e.tensor_scalar(ACC[:], ABUF[:, 0:512], PFM[:, O_CAW + c * 31:O_CAW + c * 31 + 1],
                                                    PFM[:, O_CAB + c:O_CAB + c + 1], ALU.mult, ALU.add),
                     R=[bAB, self.bPFM], W=[bACC])
            for k in range(1, 31):
                self.dve(lambda e, c=c, k=k: e.scalar_tensor_tensor(
                    ACC[:], ABUF[:, k:k + 512], PFM[:, O_CAW + c * 31 + k:O_CAW + c * 31 + k + 1], ACC[:],
                    ALU.mult, ALU.add), R=[bAB, bACC, self.bPFM], W=[bACC])
            self.act(lambda e, c=c: e.activation(out=A[:, c, :], in_=ACC[:], func=AF.Copy), R=[bACC], W=[bA[c]])
            sq = c % 2
            self.act(lambda e, sq=sq: e.activation(out=SQ[:, sq, :], in_=ACC[:], func=AF.Square), R=[bACC], W=[bSQ[sq]])

            def st1(e, c=c):
                return e.matmul(psS1[:], self.ONES[:], A[:, c, :], start=(c == 0), stop=(c == 7))

            def st2(e, c=c, sq=sq):
                return e.matmul(psS2[:], self.ONES[:], SQ[:, sq, :], start=(c == 0), stop=(c == 7))

            P.op("pe", st1, R=[bA[c], self.bONES], W=[bpsS1])
            P.op("pe", st2, R=[bSQ[sq], self.bONES], W=[bpsS2])

            ps_gc, bps_gc = inproj(24 + c)
            ps_v, bps_v = inproj(32 + c)
            ps_gb, bps_gb = inproj(16 + c)
            self.act(lambda e, ps_gc=ps_gc, c=c: e.activation(out=GC[:], in_=ps_gc[:], func=AF.Identity,
                                                              bias=bias(O_BIN, 24 + c), scale=1.0),
                     R=[bps_gc, self.bPFM], W=[bGC])
            if tt == 0:
                self.dve(lambda e: e.memset(GV[:, 0:2], 0.0), R=[], W=[bGV])
            else:
                self.dve(lambda e, c=c: e.tensor_copy(GV[:, 0:2], self.HBv[:, c, :]), R=[self.bHB[c]], W=[bGV])
            self.dve(lambda e, ps_v=ps_v, c=c: e.scalar_tensor_tensor(GV[:, 2:514], ps_v[:], bias(O_BIN, 32 + c), GC[:],
                                                                      ALU.add, ALU.mult),
                     R=[bps_v, bGC, self.bPFM], W=[bGV])
            if tt < 3:
                self.act(lambda e, c=c: e.activation(out=self.HBv[:, c, :], in_=GV[:, 512:514], func=AF.Copy),
                         R=[bGV], W=[self.bHB[c]])
            self.dve(lambda e, c=c: e.tensor_scalar(U[:], GV[:, 0:512], PFM[:, O_CBW + c * 3:O_CBW + c * 3 + 1], None,
                                                    ALU.mult), R=[bGV, self.bPFM], W=[bU])
            for k in (1, 2):
                self.dve(lambda e, c=c, k=k: e.scalar_tensor_tensor(
                    U[:], GV[:, k:k + 512], PFM[:, O_CBW + c * 3 + k:O_CBW + c * 3 + k + 1], U[:], ALU.mult, ALU.add),
                    R=[bGV, bU, self.bPFM], W=[bU])
            self.dve(lambda e, ps_gb=ps_gb, c=c: e.scalar_tensor_tensor(YB[:, c, :], ps_gb[:], bias(O_BIN, 16 + c), U[:],
                                                                        ALU.add, ALU.mult),
                     R=[bps_gb, bU, self.bPFM], W=[bYB[c]])

            ps_p, bps_p = inproj(40 + c)
            g = c // 2
            win = 2 << g
            if tt == 0:
                self.dve(lambda e: e.memset(PIN[:, 0:15], 0.0), R=[], W=[bPIN])
            else:
                self.dve(lambda e, c=c: e.tensor_copy(PIN[:, 0:15], self.HC[:, c, 0:15]), R=[self.bHC[c]], W=[bPIN])
            self.act(lambda e, ps_p=ps_p, c=c: e.activation(out=PIN[:, 15:527], in_=ps_p[:], func=AF.Identity,
                                                            bias=bias(O_BIN, 40 + c), scale=1.0),
                     R=[bps_p, self.bPFM], W=[bPIN])
            if tt < 3:
                self.act(lambda e, c=c: e.activation(out=self.HC[:, c, 0:15], in_=PIN[:, 512:527], func=AF.Copy),
                         R=[bPIN], W=[self.bHC[c]])
            src, bsrc = PIN, bPIN
            sh = 1
            pp = 0
            while sh < win:
                dst, bdst = (S0, bS0) if pp == 0 else (S1, bS1)
                lo = 2 * sh - 1
                self.dve(lambda e, src=src, dst=dst, sh=sh, lo=lo: e.tensor_tensor(
                    dst[:, lo:527], src[:, lo:527], src[:, lo - sh:527 - sh], ALU.add), R=[bsrc], W=[bdst])
                src, bsrc = dst, bdst
                sh *= 2
                pp ^= 1
            self.dve(lambda e, src=src, c=c, win=win: e.scalar_tensor_tensor(
                PL[:, c, :], src[:, 15:527], 1.0 / win, PIN[:, 15:527], ALU.mult, ALU.subtract),
                R=[bsrc, bPIN], W=[bPL[c]])
            if tt == 0:
                n = win - 1
                self.dve(lambda e, src=src, n=n: e.tensor_tensor(XH[:, 0:n], src[:, 15:15 + n], self.RC15[:, 0:n], ALU.mult),
                         R=[bsrc, self.bRC], W=[bXH])
                self.dve(lambda e, c=c, n=n: e.tensor_tensor(PL[:, c, 0:n], XH[:, 0:n], PIN[:, 15:15 + n], ALU.subtract),
                         R=[bXH, bPIN], W=[bPL[c]])

        self.dve(lambda e: e.tensor_scalar(MEANB[:], psS1[:], 1.0 / D, None, ALU.mult), R=[bpsS1], W=[bMEANB])
        self.dve(lambda e: e.tensor_tensor(XH[:], MEANB[:], MEANB[:], ALU.mult), R=[bMEANB], W=[bXH])
        self.dve(lambda e: e.scalar_tensor_tensor(RSTDB[:], psS2[:], 1.0 / D, XH[:], ALU.mult, ALU.subtract),
                 R=[bpsS2, bXH], W=[bRSTDB])
        self.act(lambda e: e.activation(out=RSTDB[:], in_=RSTDB[:], func=AF.Sqrt, bias=self.EPSC[:, 0:1], scale=1.0),
                 R=[bRSTDB, self.bRC], W=[bRSTDB])
        self.dve(lambda e: e.reciprocal(RSTDB[:], RSTDB[:]), R=[bRSTDB], W=[bRSTDB])
        for c in range(8):
            self.dve(lambda e, c=c: e.tensor_tensor(XH[:], A[:, c, :], MEANB[:], ALU.subtract), R=[bA[c], bMEANB], W=[bXH])
            self.dve(lambda e: e.tensor_tensor(XH[:], XH[:], RSTDB[:], ALU.mult), R=[bXH, bRSTDB], W=[bXH])
            self.act(lambda e, c=c: e.activation(out=A[:, c, :], in_=XH[:], func=AF.Silu,
                                                 bias=PFM[:, O_LAB + c:O_LAB + c + 1], scale=PFM[:, O_LAG + c:O_LAG + c + 1]),
                     R=[bXH, self.bPFM], W=[bA[c]])
        self.ps_n = 8

        keep = self.scr_off_keep = 3 * 8 * 512 * 2
        self.P.phase_barrier(self.DUMMY[:, 0:1])
        for b in bA + bYB + bPL:
            self.P.scratch_live.append(b)
        self.scr_off = keep
        M, bM = self.carve("M", [8, 512], BF16, nbuf=8)
        SG, bSG = self.carve("SG", [3, 512], F32, nbuf=3)
        T1, bT1 = self.carve("T1", [512], F32)
        T2, bT2 = self.carve("T2", [512], F32)
        for d in range(8):
            Wa, bWa = self.load_ws(self.wcols(self.w_a_out[l], d * 128, 128))
            ps_a, bps_a = self.ps_next()
            self.mm(ps_a[:], [(Wa[:, k, :], A[:, k, :]) for k in range(8)], R=[bWa] + bA, bps=bps_a)
            Wb, bWb = self.load_ws(self.wcols(self.w_b_out[l], d * 128, 128))
            ps_b, bps_b = self.ps_next()
            self.mm(ps_b[:], [(Wb[:, k, :], YB[:, k, :]) for k in range(8)], R=[bWb] + bYB, bps=bps_b)
            g, dd = d // 2, d % 2
            ps_c, bps_c = self.ps_next()
            self.mm(ps_c[:], [(self.PW[:, g, cc, dd * 128:(dd + 1) * 128], PL[:, 2 * g + cc, :]) for cc in range(2)],
                    R=[self.bPW, bPL[2 * g], bPL[2 * g + 1]], bps=bps_c)
            gates = []
            for br in range(3):
                ps_x, bps_x = inproj(48 + 8 * br + d)
                self.act(lambda e, ps_x=ps_x, br=br, d=d: e.activation(out=SG[:, br, :], in_=ps_x[:], func=AF.Sigmoid,
                                                                       bias=bias(O_BIN, 48 + 8 * br + d), scale=1.0),
                         R=[bps_x, self.bPFM], W=[bSG[br]])
            self.dve(lambda e, ps_a=ps_a, d=d: e.scalar_tensor_tensor(T1[:], ps_a[:], bias(O_BAO, d), SG[:, 0, :],
                                                                      ALU.add, ALU.mult),
                     R=[bps_a, bSG[0], self.bPFM], W=[bT1])
            self.dve(lambda e, ps_b=ps_b: e.tensor_tensor(T2[:], ps_b[:], SG[:, 1, :], ALU.mult), R=[bps_b, bSG[1]], W=[bT2])
            self.dve(lambda e: e.tensor_tensor(T1[:], T1[:], T2[:], ALU.add), R=[bT1, bT2], W=[bT1])
            self.dve(lambda e, ps_c=ps_c, d=d: e.scalar_tensor_tensor(T2[:], ps_c[:], bias(O_PSC, d), SG[:, 2, :],
                                                                      ALU.mult, ALU.mult),
                     R=[bps_c, bSG[2], self.bPFM], W=[bT2])
            self.dve(lambda e, d=d: e.tensor_tensor(M[:, d, :], T1[:], T2[:], ALU.add), R=[bT1, bT2], W=[bM[d]])

        if tt == 0:
            self.load_gb(self.rows[l, 0], self.rows[l, 1])
        for half in range(2):
            Wm, bWm = self.load_wl(self.wcols(self.w_mix[l], half * 512, 512))
            for s in range(4):
                ps, bps = self.ps_next()
                self.mm(ps[:], [(M[:, k, s * 128:(s + 1) * 128], Wm[:, k, :]) for k in range(8)], R=[bWm] + bM, bps=bps)
                self.resid(4 * tt + s, half, ps[:], bps)
        for s in range(4):
            self.ln_x(4 * tt + s)

    def attn_kv(self, l):
        self.new_phase()
        self.KT, self.bKT = self.carve("KT", [8, MEM], BF16)
        self.V, self.bV = self.carve("V", [2, D], BF16)
        self.attn_keep = self.scr_off
        KT, bKT, V, bV = self.KT, self.bKT, self.V, self.bV
        for f in range(8):
            W, bW = self.load_ws(self.wcols(self.w_xk[l], f * 128, 128))
            ps, bps = self.ps_next()
            self.mm(ps[:, 0:MEM], [(W[:, k, :], self.MEMT[:, k, :]) for k in range(8)], R=[bW] + self.bMEMT, bps=bps)
            self.act(lambda e, ps=ps, f=f: e.activation(out=KT[:, f, :], in_=ps[:, 0:MEM], func=AF.Copy), R=[bps], W=[bKT])
        for half in range(2):
            W, bW = self.load_wl(self.wcols(self.w_xv[l], half * 512, 512))
            for mc in range(2):
                ps, bps = self.ps_next()
                self.mm(ps[:], [(self.MEMT[:, k, mc * 128:(mc + 1) * 128], W[:, k, :]) for k in range(8)],
                        R=[bW] + self.bMEMT, bps=bps)
                self.act(lambda e, ps=ps, mc=mc, half=half: e.activation(out=V[:, mc, half * 512:(half + 1) * 512],
                                                                         in_=ps[:], func=AF.Copy), R=[bps], W=[bV])
        self.load_gb(self.rows[l, 2], self.rows[l, 3])

    def attn_tile(self, l, tt):
        P = self.P
        KT, bKT, V, bV = self.KT, self.bKT, self.V, self.bV
        xt_bufs = [self.bXT[4 * tt + s] for s in range(4)]
        tsl = slice(tt * 512, (tt + 1) * 512)
        self.P.phase_barrier(self.DUMMY[:, 0:1])
        self.P.scratch_live.extend([bKT, bV])
        self.scr_off = self.attn_keep
        QT, bQT = self.carve("QT", [8, 512], BF16, nbuf=8)
        OT, bOT = self.carve("OT", [8, 512], BF16, nbuf=8)
        PT, bPT = self.carve("PT", [2, 2, 512], BF16, nbuf=2)
        PF, bPF = self.carve("PF", [2, MEM], F32, nbuf=2)
        PN, bPN = self.carve("PN", [2, MEM], BF16, nbuf=2)
        SM, bSM = self.carve("SM", [4, 4], F32, nbuf=4)
        for f in range(8):
            W, bW = self.load_ws(self.wcols(self.w_xq[l], f * 128, 128))
            ps, bps = self.ps_next()
            self.mm(ps[:], [(W[:, k, :], self.XT[:, k, tsl]) for k in range(8)], R=[bW] + xt_bufs, bps=bps)
            self.act(lambda e, ps=ps, f=f: e.activation(out=QT[:, f, :], in_=ps[:], func=AF.Copy), R=[bps], W=[bQT[f]])
        it = 0
        for h in range(4):
            pt = h % 2
            for s in range(4):
                r = it % 2
                q = it % 4
                it += 1
                ps, bps = self.ps_next()
                self.mm(ps[:, 0:MEM], [(QT[:, 2 * h + kk, s * 128:(s + 1) * 128], KT[:, 2 * h + kk, :]) for kk in range(2)],
                        R=[bQT[2 * h], bQT[2 * h + 1], bKT], bps=bps)
                self.dve(lambda e, ps=ps, q=q: e.reduce_max(SM[:, q, 0:1], ps[:, 0:MEM], AX.X), R=[bps], W=[bSM[q]])
                self.dve(lambda e, q=q: e.tensor_scalar(SM[:, q, 1:2], SM[:, q, 0:1], -1.0 / 16.0, None, ALU.mult),
                         R=[bSM[q]], W=[bSM[q]])
                self.act(lambda e, ps=ps, r=r, q=q: e.activation(out=PF[:, r, :], in_=ps[:, 0:MEM], func=AF.Exp,
                                                                 bias=SM[:, q, 1:2], scale=1.0 / 16.0),
                         R=[bps, bSM[q]], W=[bPF[r]])
                self.dve(lambda e, r=r, q=q: e.reduce_sum(SM[:, q, 2:3], PF[:, r, :], AX.X), R=[bPF[r]], W=[bSM[q]])
                self.dve(lambda e, q=q: e.reciprocal(SM[:, q, 3:4], SM[:, q, 2:3]), R=[bSM[q]], W=[bSM[q]])
                self.dve(lambda e, r=r, q=q: e.tensor_scalar(PN[:, r, :], PF[:, r, :], SM[:, q, 3:4], None, ALU.mult),
                         R=[bPF[r], bSM[q]], W=[bPN[r]])
                ps2, bps2 = self.ps_next()
                ps2b = ps2[:].bitcast(BF16)

                def tr(e, r=r, ps2b=ps2b):
                    for mc in range(2):
                        ins = e.transpose(ps2b[:, mc * 128:(mc + 1) * 128], PN[:, r, mc * 128:(mc + 1) * 128], self.IDENT[:])
                    return ins

                P.op("pe", tr, R=[bPN[r], self.bID], W=[bps2])
                self.act(lambda e, ps2b=ps2b, pt=pt, s=s: e.activation(
                    out=PT[:, pt, :, s * 128:(s + 1) * 128], in_=ps2b[:, 0:256].rearrange("p (m t) -> p m t", m=2),
                    func=AF.Copy), R=[bps2], W=[bPT[pt]])
            for kk in range(2):
                ps, bps = self.ps_next()
                self.mm(ps[:], [(V[:, mc, (2 * h + kk) * 128:(2 * h + kk + 1) * 128], PT[:, pt, mc, :]) for mc in range(2)],
                        R=[bV, bPT[pt]], bps=bps)
                self.act(lambda e, ps=ps, h=h, kk=kk: e.activation(out=OT[:, 2 * h + kk, :], in_=ps[:], func=AF.Copy),
                         R=[bps], W=[bOT[2 * h + kk]])
        for half in range(2):
            Wo, bWo = self.load_wl(self.wcols(self.w_xo[l], half * 512, 512))
            for s in range(4):
                ps, bps = self.ps_next()
                self.mm(ps[:], [(OT[:, k, s * 128:(s + 1) * 128], Wo[:, k, :]) for k in range(8)], R=[bWo] + bOT, bps=bps)
                self.resid(4 * tt + s, half, ps[:], bps)
        for s in range(4):
            self.ln_x(4 * tt + s)

    def moe(self, l):
        P = self.P
        PFM = self.PFM
        self.new_phase()
        HT, bHT = self.carve("HT", [8, T], BF16, nbuf=8)
        G2, bG2 = self.carve("G2", [16, NE], F32, nbuf=16)
        GT, bGT = self.carve("GT", [T], BF16, nbuf=16)
        LG, bLG = self.carve("LG", [2, NE], F32, nbuf=2)
        EX, bEX = self.carve("EX", [2, NE], F32, nbuf=2)
        M8, bM8 = self.carve("M8", [2, 12], F32, nbuf=2)
        GBF, bGBF = self.carve("GBF", [2, NE], BF16, nbuf=2)
        GCt, bGCt = self.carve("GCt", [2, 512], F32, nbuf=2)
        UCt, bUCt = self.carve("UCt", [2, 512], F32, nbuf=2)
        TS, bTS, RL, bRL = GCt, bGCt, UCt, bUCt
        for i in range(16):
            r = i % 2
            ps, bps = self.ps_next()
            self.mm(ps[:, 0:NE], [(self.XT[:, k, i * 128:(i + 1) * 128], self.RW[:, k, :]) for k in range(8)],
                    R=[self.bXT[i], self.bRW], bps=bps)
            self.dve(lambda e, ps=ps, r=r: e.tensor_tensor(LG[:, r, :], ps[:, 0:NE], self.RB[:], ALU.add),
                     R=[bps, self.bRB], W=[bLG[r]])
            self.dve(lambda e, r=r: e.max(M8[:, r, 0:8], LG[:, r, :]), R=[bLG[r]], W=[bM8[r]])
            self.dve(lambda e, r=r: e.tensor_scalar(M8[:, r, 8:9], M8[:, r, 0:1], -1.0, None, ALU.mult), R=[bM8[r]], W=[bM8[r]])
            self.act(lambda e, r=r: e.activation(out=EX[:, r, :], in_=LG[:, r, :], func=AF.Exp, bias=M8[:, r, 8:9], scale=1.0),
                     R=[bLG[r], bM8[r]], W=[bEX[r]])
            self.dve(lambda e, r=r: e.tensor_scalar(LG[:, r, :], LG[:, r, :], M8[:, r, 3:4], None, ALU.is_ge),
                     R=[bLG[r], bM8[r]], W=[bLG[r]])
            self.dve(lambda e, r=r: e.tensor_tensor(EX[:, r, :], EX[:, r, :], LG[:, r, :], ALU.mult), R=[bEX[r], bLG[r]], W=[bEX[r]])
            self.dve(lambda e, r=r: e.reduce_sum(M8[:, r, 9:10], EX[:, r, :], AX.X), R=[bEX[r]], W=[bM8[r]])
            self.dve(lambda e, r=r: e.reciprocal(M8[:, r, 10:11], M8[:, r, 9:10]), R=[bM8[r]], W=[bM8[r]])
            self.dve(lambda e, r=r: e.tensor_scalar(GBF[:, r, :], EX[:, r, :], M8[:, r, 10:11], None, ALU.mult),
                     R=[bEX[r], bM8[r]], W=[bGBF[r]])
            self.dve(lambda e, r=r, i=i: e.tensor_scalar(G2[:, i, :], EX[:, r, :], M8[:, r, 10:11], 1.0 / 1.702,
                                                         ALU.mult, ALU.mult), R=[bEX[r], bM8[r]], W=[bG2[i]])
            ps2, bps2 = self.ps_next()
            ps2b = ps2[:].bitcast(BF16)
            P.op("pe", lambda e, r=r, ps2b=ps2b: e.transpose(ps2b[0:NE, 0:128], GBF[:, r, :], self.IDENT[:]),
                 R=[bGBF[r], self.bID], W=[bps2])
            self.act(lambda e, ps2b=ps2b, i=i: e.activation(out=GT[0:NE, i * 128:(i + 1) * 128], in_=ps2b[0:NE, 0:128],
                                                            func=AF.Copy), R=[bps2], W=[bGT[i]])
        for i in range(16):
            for half in range(2):
                ps, bps = self.ps_next()
                self.mm(ps[:], [(GT[0:NE, i * 128:(i + 1) * 128], self.BD[:, half * 512:(half + 1) * 512])],
                        R=[bGT[i], self.bBD], bps=bps)
                self.resid(i, half, ps[:], bps)
        it = 0
        for ex in range(self.ne):
            wgu = self.w_gu[l, ex]
            for j in range(8):
                Wg, bWg = self.load_ws(self.wcols(wgu, j * 128, 128))
                Wu, bWu = self.load_ws(self.wcols(wgu, D + j * 128, 128))
                bg = PFM[:, O_BGU + ex * 16 + j:O_BGU + ex * 16 + j + 1]
                bu = PFM[:, O_BGU + ex * 16 + 8 + j:O_BGU + ex * 16 + 8 + j + 1]
                for tt in range(4):
                    r = it % 2
                    it += 1
                    tsl = slice(tt * 512, (tt + 1) * 512)
                    xt_bufs = [self.bXT[4 * tt + s] for s in range(4)]
                    psg, bpsg = self.ps_next()
                    self.mm(psg[:], [(Wg[:, k, :], self.XT[:, k, tsl]) for k in range(8)], R=[bWg] + xt_bufs, bps=bpsg)
                    psu, bpsu = self.ps_next()
                    self.mm(psu[:], [(Wu[:, k, :], self.XT[:, k, tsl]) for k in range(8)], R=[bWu] + xt_bufs, bps=bpsu)
                    self.dve(lambda e, psg=psg, bg=bg, r=r: e.tensor_scalar(GCt[:, r, :], psg[:], bg, 7.0, ALU.add, ALU.min),
                             R=[bpsg, self.bPFM], W=[bGCt[r]])
                    self.act(lambda e, r=r: e.activation(out=TS[:, r, :], in_=GCt[:, r, :], func=AF.Silu, scale=1.702),
                             R=[bGCt[r]], W=[bTS[r]])
                    self.dve(lambda e, psu=psu, bu=bu, r=r: e.tensor_scalar(UCt[:, r, :], psu[:], bu, 7.0, ALU.add, ALU.min),
                             R=[bpsu, self.bPFM], W=[bUCt[r]])
                    self.act(lambda e, r=r: e.activation(out=RL[:, r, :], in_=UCt[:, r, :], func=AF.Relu, bias=7.0, scale=1.0),
                             R=[bUCt[r]], W=[bRL[r]])
                    self.dve(lambda e, r=r, j=j, tsl=tsl: e.scalar_tensor_tensor(HT[:, j, tsl], RL[:, r, :], -6.0, TS[:, r, :],
                                                                                 ALU.add, ALU.mult),
                             R=[bRL[r], bTS[r]], W=[bHT[j]])
            for half in range(2):
                Wd, bWd = self.load_wl(self.w_down[l, ex].rearrange("(kc p) f -> p kc f", p=128)[:, :, half * 512:(half + 1) * 512])
                for i in range(16):
                    ps, bps = self.ps_next()
                    self.mm(ps[:], [(HT[:, k, i * 128:(i + 1) * 128], Wd[:, k, :]) for k in range(8)], R=[bWd] + bHT, bps=bps)
                    xs = self.X[:, i, half * 512:(half + 1) * 512]
                    self.dve(lambda e, ps=ps, xs=xs, i=i, ex=ex: e.scalar_tensor_tensor(
                        xs, ps[:], G2[:, i, ex:ex + 1], xs, ALU.mult, ALU.add),
                        R=[bps, bG2[i], self.bX[i]], W=[self.bX[i]])
        self.load_gb(self.rows[l, 4], self.rows[l, 5])
        for i in range(16):
            self.ln_x(i)

    def build(self):
        nc = self.nc
        with nc.Fori(0, self.NS) as s:
            self.setup_consts()
            self.load_seq(s)
            for l in range(self.L):
                self.load_layer_params(l)
                if self.do_mixer:
                    for tt in range(4):
                        self.mixer_tile(l, tt)
                if self.do_attn:
                    self.attn_kv(l)
                    for tt in range(4):
                        self.attn_tile(l, tt)
                if self.do_moe:
                    self.moe(l)
            self.store_seq(s)
            sems = self.P.emit(self.bX)
            nc.all_engine_barrier()
            nc.gpsimd.dma_reset()
            for sm in sems:
                nc.sync.sem_clear(sm)
            nc.all_engine_barrier()
        return nc


def _pack_pfm(inp, l):
    def fm(v):
        return np.ascontiguousarray(v.reshape(-1, 128).T)

    cols = [fm(inp["b_in"][l]),
            np.ascontiguousarray(inp["conv_a_w"][l].T.reshape(8, 128, 31).transpose(1, 0, 2).reshape(128, 248)),
            fm(inp["conv_a_b"][l]), fm(inp["ln_a_g"][l]), fm(inp["ln_a_b"][l]), fm(inp["b_a_out"][l]),
            np.ascontiguousarray(inp["conv_b_w"][l].T.reshape(8, 128, 3).transpose(1, 0, 2).reshape(128, 24)),
            fm(inp["pool_scale"][l]),
            np.ascontiguousarray(inp["b_gu"][l].reshape(NE, 16, 128).transpose(2, 0, 1).reshape(128, NE * 16))]
    out = np.concatenate(cols, axis=1).astype(np.float32)
    assert out.shape == (128, NPF)
    return out


def make_inmaps(inp, layers, n_cores, nseq, x_override=None):
    ls = list(layers)
    f32 = lambda a: np.ascontiguousarray(np.asarray(a, dtype=np.float32))
    shared = {
        "memln": f32(np.stack([inp["mem_ln_g"], inp["mem_ln_b"]])),
        "ident": np.eye(128, dtype=np.float32),
        "pfm": f32(np.stack([_pack_pfm(inp, l) for l in ls])),
        "rows": f32(np.stack([np.stack([inp["ln1_g"][l], inp["ln1_b"][l], inp["ln2_g"][l], inp["ln2_b"][l],
                                        inp["ln3_g"][l], inp["ln3_b"][l]]) for l in ls])),
    }
    for k in ("router_b", "w_in", "w_a_out", "w_b_out", "pool_w", "w_mix_out", "w_xq", "w_xk", "w_xv", "w_xo",
              "router_w", "w_gu", "w_down", "b_down"):
        a = inp[k]
        shared[k] = f32(a[ls[0]:ls[-1] + 1]) if ls == list(range(ls[0], ls[-1] + 1)) else f32(a[ls])
    x = inp["x"] if x_override is None else x_override
    maps = []
    for c in range(n_cores):
        m = dict(shared)
        m["x"] = f32(x[c * nseq:(c + 1) * nseq])
        m["mem"] = f32(inp["mem"][c * nseq:(c + 1) * nseq])
        maps.append(m)
    return maps


_NC_CACHE = {}


def _program(L, NS, **kw):
    key = (L, NS, tuple(sorted(kw.items())))
    if key not in _NC_CACHE:
        _NC_CACHE[key] = Builder(L, NS, **kw).build()
    return _NC_CACHE[key]


FUSED = True


def kernel(**inputs):
    inp = {k: np.asarray(v) for k, v in inputs.items()}
    if FUSED:
        nc = _program(DEPTH, NSEQ)
        maps = make_inmaps(inp, range(DEPTH), NCORES, NSEQ)
        res = run_bass_kernel_spmd(nc, maps, core_ids=list(range(NCORES)))
        return np.concatenate([r["y"] for r in res.results], axis=0).astype(np.float32)
    x = inp["x"]
    nc = _program(1, NSEQ)
    for l in range(DEPTH):
        maps = make_inmaps(inp, [l], NCORES, NSEQ, x_override=x)
        res = run_bass_kernel_spmd(nc, maps, core_ids=list(range(NCORES)))
        x = np.concatenate([r["y"] for r in res.results], axis=0).astype(np.float32)
    return x
```

```python
import numpy as np
import concourse.bass as bass
import concourse.mybir as mybir
from concourse.bass_utils import run_bass_kernel_spmd

F32 = mybir.dt.float32
BF16 = mybir.dt.bfloat16
AF = mybir.ActivationFunctionType
ALU = mybir.AluOpType
AX = mybir.AxisListType

D = 1024
T = 2048
MEM = 256
NE = 32
FIN = 9216
DEPTH = 4
ALPHA = float((2 * DEPTH) ** 0.25)
EPS = 1e-5
NCORES = 8
NSEQ = 4

O_BIN, O_CAW, O_CAB, O_LAG, O_LAB, O_BAO, O_CBW, O_PSC, O_BGU = 0, 72, 320, 328, 336, 344, 352, 376, 384
NPF = 896

EPOCH = 24000
ENGS = ("pe", "act", "dve", "pool", "sp")


class Buf:
    __slots__ = ("name", "writer", "readers", "dma_readers", "sem", "ndma")

    def __init__(self, name, writer=None):
        self.name = name
        self.writer = writer
        self.readers = {}
        self.dma_readers = []
        self.sem = None
        self.ndma = 0


class Op:
    __slots__ = ("eng", "fn", "deps", "is_dma", "buf", "dma_val", "needs_inc", "ev", "waits", "seq")

    def __init__(self, eng, fn):
        self.eng = eng
        self.fn = fn
        self.deps = []
        self.is_dma = False
        self.buf = None
        self.dma_val = 0
        self.needs_inc = False
        self.ev = None
        self.waits = None
        self.seq = 0


class Prog:
    def __init__(self, nc):
        self.nc = nc
        self.streams = {e: [] for e in ENGS}
        self.nops = 0
        self.bufs = []
        self.scratch_live = []
        self.barrier_op = None

    def buf(self, name, scratch=False):
        b = Buf(name, self.barrier_op if scratch else None)
        self.bufs.append(b)
        if scratch:
            self.scratch_live.append(b)
        return b

    def op(self, eng, fn, R=(), W=(), dma=None, ndma=1):
        o = Op(eng, fn)
        self.nops += 1
        o.seq = self.nops
        deps = {}

        def add(d):
            if d is None or d is o:
                return
            if d.is_dma:
                deps[id(d)] = d
                return
            if d.eng == "pe" and eng == "pe":
                return
            k = d.eng
            if k not in deps or deps[k].seq < d.seq:
                deps[k] = d

        for b in R:
            add(b.writer)
        for b in W:
            add(b.writer)
            for r in b.readers.values():
                add(r)
            for r in b.dma_readers:
                add(r)
        o.deps = list(deps.values())
        for d in o.deps:
            if not d.is_dma:
                d.needs_inc = True
        if dma is not None:
            o.is_dma = True
            o.buf = dma
            dma.ndma += ndma
            o.dma_val = 16 * dma.ndma
        for b in W:
            b.writer = o
            b.readers = {}
            b.dma_readers = []
        for b in R:
            if b.writer is o:
                continue
            if o.is_dma:
                b.dma_readers.append(o)
            else:
                b.readers[eng] = o
        self.streams[eng].append(o)
        return o

    def phase_barrier(self, dummy_ap):
        live = self.scratch_live
        self.scratch_live = []
        tok = Buf("phase_tok")
        o = self.op("dve", lambda e: e.memset(dummy_ap, 0.0), R=(), W=live + [tok])
        o.needs_inc = True
        self.barrier_op = o
        return o

    def emit(self, final_bufs):
        nc = self.nc
        sems = {}

        def get_sem(key):
            if key not in sems:
                sems[key] = nc.alloc_semaphore("s_%s_%d" % key)
            return sems[key]

        for e in ENGS:
            cnt = 0
            for o in self.streams[e]:
                if o.is_dma:
                    if o.buf.sem is None:
                        o.buf.sem = nc.alloc_semaphore("d%d_%s" % (len(sems) + o.seq, o.buf.name))
                    continue
                if o.needs_inc:
                    ep, v = divmod(cnt, EPOCH)
                    o.ev = (e, ep, v + 1)
                    cnt += 1
        for e in ENGS:
            seen_c = {}
            seen_d = {}
            for o in self.streams[e]:
                w = []
                for d in o.deps:
                    if d.is_dma:
                        s = d.buf.sem
                        if seen_d.get(id(s), 0) < d.dma_val:
                            seen_d[id(s)] = d.dma_val
                            w.append((s, d.dma_val))
                    else:
                        pe, ep, v = d.ev
                        if seen_c.get(pe, (-1, 0)) < (ep, v):
                            seen_c[pe] = (ep, v)
                            w.append((get_sem((pe, ep)), v))
                o.waits = w
        engobj = {"pe": nc.tensor, "act": nc.scalar, "dve": nc.vector, "pool": nc.gpsimd, "sp": nc.sync}
        for e in ENGS:
            eng = engobj[e]
            for o in self.streams[e]:
                for (s, v) in o.waits:
                    eng.wait_ge(s, v)
                r = o.fn(eng)
                if o.is_dma:
                    if not isinstance(r, (list, tuple)):
                        r = [r]
                    for ins in r:
                        ins.then_inc(o.buf.sem, 16)
                elif o.needs_inc:
                    pe, ep, v = o.ev
                    r.then_inc(get_sem((pe, ep)), 1)
        dma_sems = [b.sem for b in self.bufs if b.sem is not None]
        for b in self.bufs:
            if b.sem is not None:
                nc.sync.wait_ge(b.sem, 16 * b.ndma)
        return list(sems.values()) + dma_sems


class Builder:
    def __init__(self, L, NS, do_mixer=True, do_attn=True, do_moe=True, ne=NE):
        self.L, self.NS = L, NS
        self.do_mixer, self.do_attn, self.do_moe, self.ne = do_mixer, do_attn, do_moe, ne
        nc = self.nc = bass.Bass("TRN2", target_bir_lowering=False)
        P = self.P = Prog(nc)

        def din(name, shape):
            return nc.dram_tensor(name, list(shape), F32, kind="ExternalInput").ap()

        self.x = din("x", [NS, T, D])
        self.mem = din("mem", [NS, MEM, D])
        self.memln = din("memln", [2, D])
        self.ident_d = din("ident", [128, 128])
        self.pfm = din("pfm", [L, 128, NPF])
        self.rows = din("rows", [L, 6, D])
        self.router_b = din("router_b", [L, NE])
        self.w_in = din("w_in", [L, D, FIN])
        self.w_a_out = din("w_a_out", [L, D, D])
        self.w_b_out = din("w_b_out", [L, D, D])
        self.pool_w = din("pool_w", [L, 4, 256, 256])
        self.w_mix = din("w_mix_out", [L, D, D])
        self.w_xq = din("w_xq", [L, D, D])
        self.w_xk = din("w_xk", [L, D, D])
        self.w_xv = din("w_xv", [L, D, D])
        self.w_xo = din("w_xo", [L, D, D])
        self.router_w = din("router_w", [L, D, NE])
        self.w_gu = din("w_gu", [L, NE, D, 2 * D])
        self.w_down = din("w_down", [L, NE, D, D])
        self.b_down = din("b_down", [L, NE, D])
        self.y = nc.dram_tensor("y", [NS, T, D], F32, kind="ExternalOutput").ap()

        def sb(name, shape, dt):
            return nc.alloc_sbuf_tensor(name, list(shape), dt)

        self.X = sb("X", [128, 16, D], F32)
        self.bX = [P.buf("X%d" % i) for i in range(16)]
        self.XT = sb("XT", [128, 8, T], BF16)
        self.bXT = [P.buf("XT%d" % i) for i in range(16)]
        self.MEMT = sb("MEMT", [128, 8, MEM], BF16)
        self.bMEMT = [P.buf("MEMT%d" % i) for i in range(2)]
        self.GB = sb("GB", [128, 2, D], F32)
        self.bGB = P.buf("GB")
        self.XB = [sb("XB%d" % i, [128, D], BF16) for i in range(2)]
        self.bXB = [P.buf("XB%d" % i) for i in range(2)]
        self.xb_i = 0
        self.IDF = sb("IDF", [128, 128], F32)
        self.IDENT = sb("IDENT", [128, 128], BF16)
        self.bID = P.buf("ident")
        self.ONES = sb("ONES", [128, 128], BF16)
        self.bONES = P.buf("ones")
        self.RC15 = sb("RC15", [128, 16], F32)
        self.bRC = P.buf("rc15")
        self.EPSC = sb("EPSC", [128, 1], F32)
        self.PFM = sb("PFM", [128, NPF], F32)
        self.bPFM = P.buf("pfm")
        self.PW = sb("PW", [128, 4, 2, 256], BF16)
        self.bPW = P.buf("pw")
        self.RW = sb("RW", [128, 8, NE], BF16)
        self.bRW = P.buf("rw")
        self.RB = sb("RB", [128, NE], F32)
        self.bRB = P.buf("rb")
        self.BD = sb("BD", [NE, D], BF16)
        self.bBD = P.buf("bd")
        self.HA = sb("HA", [128, 8, 30], F32)
        self.HBv = sb("HBv", [128, 8, 2], F32)
        self.HC = sb("HC", [128, 8, 16], F32)
        self.bHA = [P.buf("HA%d" % c) for c in range(8)]
        self.bHB = [P.buf("HB%d" % c) for c in range(8)]
        self.bHC = [P.buf("HC%d" % c) for c in range(8)]
        self.DUMMY = sb("DUMMY", [128, 8], F32)
        self.NST = 4
        self.BNS = [sb("BNS%d" % i, [128, 2, 6], F32) for i in range(self.NST)]
        self.MV = [sb("MV%d" % i, [128, 4], F32) for i in range(self.NST)]
        self.bST = [P.buf("ST%d" % i) for i in range(self.NST)]
        self.st_i = 0
        self.NWS = 6
        self.WS = [sb("WS%d" % i, [128, 8, 128], BF16) for i in range(self.NWS)]
        self.bWS = [P.buf("WS%d" % i) for i in range(self.NWS)]
        self.ws_i = 0
        self.NWL = 2
        self.WL = [sb("WL%d" % i, [128, 8, 512], BF16) for i in range(self.NWL)]
        self.bWL = [P.buf("WL%d" % i) for i in range(self.NWL)]
        self.wl_i = 0
        self.PS = [nc.alloc_psum_tensor("ps%d" % i, [128, 512], F32) for i in range(8)]
        self.bPS = [P.buf("ps%d" % i) for i in range(8)]
        self.ps_i = 0
        self.ps_n = 8
        self.SCR_BYTES = (nc.sbuf_bytes_remaining - 1024) // 64 * 64
        assert self.SCR_BYTES >= 52 * 1024, self.SCR_BYTES
        self.SCR = sb("SCR", [128, self.SCR_BYTES // 2], BF16)
        self.scr_off = 0

    def carve(self, name, free_shape, dt, nbuf=1):
        n = int(np.prod(free_shape))
        nbytes = n * (4 if dt == F32 else 2)
        nbytes_al = (nbytes + 31) // 32 * 32
        assert self.scr_off + nbytes_al <= self.SCR_BYTES, (name, self.scr_off, nbytes_al)
        ap = self.SCR[:, self.scr_off // 2:(self.scr_off + nbytes) // 2]
        self.scr_off += nbytes_al
        if dt == F32:
            ap = ap.bitcast(F32)
        if len(free_shape) == 2:
            ap = ap.rearrange("p (a b) -> p a b", a=free_shape[0])
        elif len(free_shape) == 3:
            ap = ap.rearrange("p (a b c) -> p a b c", a=free_shape[0], b=free_shape[1])
        bufs = [self.P.buf(name + str(i), scratch=True) for i in range(nbuf)]
        return ap, (bufs[0] if nbuf == 1 else bufs)

    def new_phase(self):
        self.P.phase_barrier(self.DUMMY[:, 0:1])
        self.scr_off = 0

    def ps_next(self):
        i = self.ps_i % self.ps_n
        self.ps_i += 1
        return self.PS[i], self.bPS[i]

    def wcols(self, w2d, c0, n):
        return w2d.rearrange("(kc p) f -> p kc f", p=128)[:, :, c0:c0 + n]

    def load_ws(self, src):
        s = self.ws_i % self.NWS
        self.ws_i += 1
        W, b = self.WS[s], self.bWS[s]
        self.P.op("pool", lambda e: e.dma_start(out=W[:], in_=src), W=[b], dma=b)
        return W, b

    def load_wl(self, src):
        s = self.wl_i % self.NWL
        self.wl_i += 1
        W, b = self.WL[s], self.bWL[s]
        self.P.op("pool", lambda e: e.dma_start(out=W[:], in_=src), W=[b], dma=b)
        return W, b

    def mm(self, out_ap, pairs, R, bps):
        pairs = list(pairs)

        def fn(e):
            n = len(pairs)
            for i, (l, r) in enumerate(pairs):
                ins = e.matmul(out_ap, l, r, start=(i == 0), stop=(i == n - 1))
            return ins

        self.P.op("pe", fn, R=R, W=[bps])

    def dve(self, fn, R, W):
        return self.P.op("dve", fn, R=R, W=W)

    def act(self, fn, R, W):
        return self.P.op("act", fn, R=R, W=W)

    def setup_consts(self):
        P = self.P
        IDF, IDENT, ONES, RC = self.IDF, self.IDENT, self.ONES, self.RC15
        P.op("sp", lambda e: e.dma_start(out=IDF[:], in_=self.ident_d), W=[self.bID], dma=self.bID)
        self.dve(lambda e: e.tensor_copy(IDENT[:], IDF[:]), R=[self.bID], W=[self.bID])
        self.dve(lambda e: e.memset(ONES[:], 1.0), R=[], W=[self.bONES])
        for t in range(16):
            self.dve(lambda e, t=t: e.memset(RC[:, t:t + 1], 1.0 / (t + 1)), R=[], W=[self.bRC])
        self.dve(lambda e: e.memset(self.EPSC[:], EPS), R=[], W=[self.bRC])

    def load_layer_params(self, l):
        P = self.P
        P.op("sp", lambda e: e.dma_start(out=self.PFM[:], in_=self.pfm[l]), W=[self.bPFM], dma=self.bPFM)
        P.op("sp", lambda e: e.dma_start(out=self.RB[:], in_=self.router_b[l].partition_broadcast(128)),
             W=[self.bRB], dma=self.bRB)
        pw_src = self.pool_w[l].rearrange("g (cc p) d -> p g cc d", p=128)

        def ld_pw(e):
            return [e.dma_start(out=self.PW[:, g], in_=pw_src[:, g]) for g in range(4)]

        P.op("pool", ld_pw, W=[self.bPW], dma=self.bPW, ndma=4)
        P.op("pool", lambda e: e.dma_start(out=self.RW[:], in_=self.wcols(self.router_w[l], 0, NE)),
             W=[self.bRW], dma=self.bRW)
        P.op("pool", lambda e: e.dma_start(out=self.BD[:], in_=self.b_down[l]), W=[self.bBD], dma=self.bBD)

    def load_gb(self, g_row, b_row):
        def f(e):
            return [e.dma_start(out=self.GB[:, 0, :], in_=g_row.partition_broadcast(128)),
                    e.dma_start(out=self.GB[:, 1, :], in_=b_row.partition_broadcast(128))]

        self.P.op("sp", f, W=[self.bGB], dma=self.bGB, ndma=2)

    def to_T(self, row_ap, brow, dstT, bdst):
        s = self.xb_i % 2
        self.xb_i += 1
        XB, bXB = self.XB[s], self.bXB[s]
        self.act(lambda e: e.activation(out=XB[:], in_=row_ap, func=AF.Copy), R=[brow], W=[bXB])
        ps, bps = self.ps_next()
        psb = ps[:].bitcast(BF16)

        def tr(e):
            for c in range(8):
                ins = e.transpose(psb[:, c * 128:(c + 1) * 128], XB[:, c * 128:(c + 1) * 128], self.IDENT[:])
            return ins

        self.P.op("pe", tr, R=[bXB, self.bID], W=[bps])
        self.act(lambda e: e.activation(out=dstT, in_=psb.rearrange("p (c t) -> p c t", c=8), func=AF.Copy),
                 R=[bps], W=[bdst])

    def ln_rows(self, row_ap, brow, dstT, bdst):
        k = self.st_i % self.NST
        self.st_i += 1
        BNS, MV, bST = self.BNS[k], self.MV[k], self.bST[k]
        self.dve(lambda e: e.bn_stats(BNS[:, 0, :], row_ap[:, 0:512]), R=[brow], W=[bST])
        self.dve(lambda e: e.bn_stats(BNS[:, 1, :], row_ap[:, 512:1024]), R=[brow], W=[bST])
        self.dve(lambda e: e.bn_aggr(MV[:, 0:2], BNS[:].rearrange("p a b -> p (a b)")), R=[bST], W=[bST])
        self.act(lambda e: e.activation(out=MV[:, 2:3], in_=MV[:, 1:2], func=AF.Sqrt, bias=self.EPSC[:, 0:1], scale=1.0),
                 R=[bST, self.bRC], W=[bST])
        self.dve(lambda e: e.reciprocal(MV[:, 2:3], MV[:, 2:3]), R=[bST], W=[bST])
        self.dve(lambda e: e.scalar_tensor_tensor(MV[:, 3:4], MV[:, 0:1], -1.0, MV[:, 2:3], ALU.mult, ALU.mult),
                 R=[bST], W=[bST])
        self.act(lambda e: e.activation(out=row_ap, in_=row_ap, func=AF.Identity, bias=MV[:, 3:4], scale=MV[:, 2:3]),
                 R=[brow, bST], W=[brow])
        self.dve(lambda e: e.tensor_tensor(row_ap, row_ap, self.GB[:, 0, :], ALU.mult), R=[brow, self.bGB], W=[brow])
        self.dve(lambda e: e.tensor_tensor(row_ap, row_ap, self.GB[:, 1, :], ALU.add), R=[brow, self.bGB], W=[brow])
        self.to_T(row_ap, brow, dstT, bdst)

    def ln_x(self, i):
        self.ln_rows(self.X[:, i, :], self.bX[i], self.XT[:, :, i * 128:(i + 1) * 128], self.bXT[i])

    def resid(self, i, half, ps_ap, bps):
        xs = self.X[:, i, half * 512:(half + 1) * 512]
        self.dve(lambda e: e.scalar_tensor_tensor(xs, xs, ALPHA, ps_ap, ALU.mult, ALU.add),
                 R=[self.bX[i], bps], W=[self.bX[i]])

    def load_seq(self, s):
        P = self.P
        for i in range(16):
            P.op("sp", lambda e, i=i: e.dma_start(out=self.X[:, i, :], in_=self.x[s, i * 128:(i + 1) * 128, :]),
                 W=[self.bX[i]], dma=self.bX[i])
        for i in range(16):
            self.to_T(self.X[:, i, :], self.bX[i], self.XT[:, :, i * 128:(i + 1) * 128], self.bXT[i])
        self.new_phase()
        MR, bMR = self.carve("MR", [2, D], F32, nbuf=2)
        self.load_gb(self.memln[0], self.memln[1])
        for mc in range(2):
            P.op("sp", lambda e, mc=mc: e.dma_start(out=MR[:, mc, :], in_=self.mem[s, mc * 128:(mc + 1) * 128, :]),
                 W=[bMR[mc]], dma=bMR[mc])
            self.ln_rows(MR[:, mc, :], bMR[mc], self.MEMT[:, :, mc * 128:(mc + 1) * 128], self.bMEMT[mc])

    def store_seq(self, s):
        for i in range(16):
            self.P.op("sp", lambda e, i=i: e.dma_start(out=self.y[s, i * 128:(i + 1) * 128, :], in_=self.X[:, i, :]),
                      R=[self.bX[i]], dma=self.bX[i])

    def mixer_tile(self, l, tt):
        P = self.P
        PFM = self.PFM
        w_in = self.w_in[l]
        xt_bufs = [self.bXT[4 * tt + s] for s in range(4)]
        tsl = slice(tt * 512, (tt + 1) * 512)

        def inproj(j):
            W, bW = self.load_ws(self.wcols(w_in, j * 128, 128))
            ps, bps = self.ps_next()
            self.mm(ps[:], [(W[:, k, :], self.XT[:, k, tsl]) for k in range(8)], R=[bW] + xt_bufs, bps=bps)
            return ps, bps

        def bias(off, j):
            return PFM[:, off + j:off + j + 1]

        self.new_phase()
        A, bA = self.carve("A", [8, 512], BF16, nbuf=8)
        YB, bYB = self.carve("YB", [8, 512], BF16, nbuf=8)
        PL, bPL = self.carve("PL", [8, 512], BF16, nbuf=8)
        GA, bGA = self.carve("GA", [512], F32)
        ABUF, bAB = self.carve("ABUF", [544], BF16)
        DG, bDG = self.carve("DG", [31, 128], BF16)
        GC, bGC = self.carve("GC", [512], F32)
        GV, bGV = self.carve("GV", [516], F32)
        U, bU = GA, bGA
        PIN, bPIN = self.carve("PIN", [528], F32)
        S0, bS0 = self.carve("S0", [528], F32)
        S1, bS1 = self.carve("S1", [528], F32)
        SQ, bSQ = self.carve("SQ", [2, 512], BF16, nbuf=2)
        MEANB, bMEANB = S0[:, 0:512], bS0
        RSTDB, bRSTDB = S1[:, 0:512], bS1
        XH, bXH = self.carve("XH", [512], F32)
        self.ps_n = 6
        psS1, bpsS1 = self.PS[6], self.bPS[6]
        psS2, bpsS2 = self.PS[7], self.bPS[7]

        for c in range(8):
            ps_l, bps_l = inproj(c)
            ps_g, bps_g = inproj(8 + c)
            self.act(lambda e, ps_g=ps_g, c=c: e.activation(out=GA[:], in_=ps_g[:], func=AF.Sigmoid,
                                                            bias=bias(O_BIN, 8 + c), scale=1.0),
                     R=[bps_g, self.bPFM], W=[bGA])
            if tt == 0:
                self.dve(lambda e: e.memset(ABUF[:, 0:30], 0.0), R=[], W=[bAB])
            else:
                self.dve(lambda e, c=c: e.tensor_copy(ABUF[:, 0:30], self.HA[:, c, :]), R=[self.bHA[c]], W=[bAB])
            self.dve(lambda e, ps_l=ps_l, c=c: e.scalar_tensor_tensor(ABUF[:, 30:542], ps_l[:], bias(O_BIN, c), GA[:],
                                                                      ALU.add, ALU.mult),
                     R=[bps_l, bGA, self.bPFM], W=[bAB])
            if tt < 3:
                self.act(lambda e, c=c: e.activation(out=self.HA[:, c, :], in_=ABUF[:, 512:542], func=AF.Copy),
                         R=[bAB], W=[self.bHA[c]])
            self.dve(lambda e, c=c: e.tensor_tensor(
                DG[:], self.IDENT[:].unsqueeze(1).broadcast_to([128, 31, 128]),
                PFM[:, O_CAW + c * 31:O_CAW + (c + 1) * 31].unsqueeze(2).broadcast_to([128, 31, 128]), ALU.mult),
                R=[self.bID, self.bPFM], W=[bDG])
            ps_cv, bps_cv = self.ps_next()
            self.mm(ps_cv[:], [(DG[:, k, :], ABUF[:, k:k + 512]) for k in range(31)], R=[bDG, bAB], bps=bps_cv)
            self.act(lambda e, c=c, ps_cv=ps_cv: e.activation(out=A[:, c, :], in_=ps_cv[:], func=AF.Identity,
                                                              bias=PFM[:, O_CAB + c:O_CAB + c + 1], scale=1.0),
                     R=[bps_cv, self.bPFM], W=[bA[c]])
            sq = c % 2
            self.act(lambda e, c=c, sq=sq, ps_cv=ps_cv: e.activation(out=SQ[:, sq, :], in_=ps_cv[:], func=AF.Square,
                                                                     bias=PFM[:, O_CAB + c:O_CAB + c + 1], scale=1.0),
                     R=[bps_cv, self.bPFM], W=[bSQ[sq]])

            def st1(e, c=c):
                return e.matmul(psS1[:], self.ONES[:], A[:, c, :], start=(c == 0), stop=(c == 7))

            def st2(e, c=c, sq=sq):
                return e.matmul(psS2[:], self.ONES[:], SQ[:, sq, :], start=(c == 0), stop=(c == 7))

            P.op("pe", st1, R=[bA[c], self.bONES], W=[bpsS1])
            P.op("pe", st2, R=[bSQ[sq], self.bONES], W=[bpsS2])

            ps_gc, bps_gc = inproj(24 + c)
            ps_v, bps_v = inproj(32 + c)
            ps_gb, bps_gb = inproj(16 + c)
            self.act(lambda e, ps_gc=ps_gc, c=c: e.activation(out=GC[:], in_=ps_gc[:], func=AF.Identity,
                                                              bias=bias(O_BIN, 24 + c), scale=1.0),
                     R=[bps_gc, self.bPFM], W=[bGC])
            if tt == 0:
                self.dve(lambda e: e.memset(GV[:, 0:2], 0.0), R=[], W=[bGV])
            else:
                self.dve(lambda e, c=c: e.tensor_copy(GV[:, 0:2], self.HBv[:, c, :]), R=[self.bHB[c]], W=[bGV])
            self.dve(lambda e, ps_v=ps_v, c=c: e.scalar_tensor_tensor(GV[:, 2:514], ps_v[:], bias(O_BIN, 32 + c), GC[:],
                                                                      ALU.add, ALU.mult),
                     R=[bps_v, bGC, self.bPFM], W=[bGV])
            if tt < 3:
                self.act(lambda e, c=c: e.activation(out=self.HBv[:, c, :], in_=GV[:, 512:514], func=AF.Copy),
                         R=[bGV], W=[self.bHB[c]])
            self.dve(lambda e, c=c: e.tensor_scalar(U[:], GV[:, 0:512], PFM[:, O_CBW + c * 3:O_CBW + c * 3 + 1], None,
                                                    ALU.mult), R=[bGV, self.bPFM], W=[bU])
            for k in (1, 2):
                self.dve(lambda e, c=c, k=k: e.scalar_tensor_tensor(
                    U[:], GV[:, k:k + 512], PFM[:, O_CBW + c * 3 + k:O_CBW + c * 3 + k + 1], U[:], ALU.mult, ALU.add),
                    R=[bGV, bU, self.bPFM], W=[bU])
            self.dve(lambda e, ps_gb=ps_gb, c=c: e.scalar_tensor_tensor(YB[:, c, :], ps_gb[:], bias(O_BIN, 16 + c), U[:],
                                                                        ALU.add, ALU.mult),
                     R=[bps_gb, bU, self.bPFM], W=[bYB[c]])

            ps_p, bps_p = inproj(40 + c)
            g = c // 2
            win = 2 << g
            if tt == 0:
                self.dve(lambda e: e.memset(PIN[:, 0:15], 0.0), R=[], W=[bPIN])
            else:
                self.dve(lambda e, c=c: e.tensor_copy(PIN[:, 0:15], self.HC[:, c, 0:15]), R=[self.bHC[c]], W=[bPIN])
            self.act(lambda e, ps_p=ps_p, c=c: e.activation(out=PIN[:, 15:527], in_=ps_p[:], func=AF.Identity,
                                                            bias=bias(O_BIN, 40 + c), scale=1.0),
                     R=[bps_p, self.bPFM], W=[bPIN])
            if tt < 3:
                self.act(lambda e, c=c: e.activation(out=self.HC[:, c, 0:15], in_=PIN[:, 512:527], func=AF.Copy),
                         R=[bPIN], W=[self.bHC[c]])
            src, bsrc = PIN, bPIN
            sh = 1
            pp = 0
            while sh < win:
                dst, bdst = (S0, bS0) if pp == 0 else (S1, bS1)
                lo = 2 * sh - 1
                self.dve(lambda e, src=src, dst=dst, sh=sh, lo=lo: e.tensor_tensor(
                    dst[:, lo:527], src[:, lo:527], src[:, lo - sh:527 - sh], ALU.add), R=[bsrc], W=[bdst])
                src, bsrc = dst, bdst
                sh *= 2
                pp ^= 1
            self.dve(lambda e, src=src, c=c, win=win: e.scalar_tensor_tensor(
                PL[:, c, :], src[:, 15:527], 1.0 / win, PIN[:, 15:527], ALU.mult, ALU.subtract),
                R=[bsrc, bPIN], W=[bPL[c]])
            if tt == 0:
                n = win - 1
                self.dve(lambda e, src=src, n=n: e.tensor_tensor(XH[:, 0:n], src[:, 15:15 + n], self.RC15[:, 0:n], ALU.mult),
                         R=[bsrc, self.bRC], W=[bXH])
                self.dve(lambda e, c=c, n=n: e.tensor_tensor(PL[:, c, 0:n], XH[:, 0:n], PIN[:, 15:15 + n], ALU.subtract),
                         R=[bXH, bPIN], W=[bPL[c]])

        self.dve(lambda e: e.tensor_scalar(MEANB[:], psS1[:], 1.0 / D, None, ALU.mult), R=[bpsS1], W=[bMEANB])
        self.dve(lambda e: e.tensor_tensor(XH[:], MEANB[:], MEANB[:], ALU.mult), R=[bMEANB], W=[bXH])
        self.dve(lambda e: e.scalar_tensor_tensor(RSTDB[:], psS2[:], 1.0 / D, XH[:], ALU.mult, ALU.subtract),
                 R=[bpsS2, bXH], W=[bRSTDB])
        self.act(lambda e: e.activation(out=RSTDB[:], in_=RSTDB[:], func=AF.Sqrt, bias=self.EPSC[:, 0:1], scale=1.0),
                 R=[bRSTDB, self.bRC], W=[bRSTDB])
        self.dve(lambda e: e.reciprocal(RSTDB[:], RSTDB[:]), R=[bRSTDB], W=[bRSTDB])
        for c in range(8):
            self.dve(lambda e, c=c: e.tensor_tensor(XH[:], A[:, c, :], MEANB[:], ALU.subtract), R=[bA[c], bMEANB], W=[bXH])
            self.dve(lambda e: e.tensor_tensor(XH[:], XH[:], RSTDB[:], ALU.mult), R=[bXH, bRSTDB], W=[bXH])
            self.act(lambda e, c=c: e.activation(out=A[:, c, :], in_=XH[:], func=AF.Silu,
                                                 bias=PFM[:, O_LAB + c:O_LAB + c + 1], scale=PFM[:, O_LAG + c:O_LAG + c + 1]),
                     R=[bXH, self.bPFM], W=[bA[c]])
        self.ps_n = 8

        keep = self.scr_off_keep = 3 * 8 * 512 * 2
        self.P.phase_barrier(self.DUMMY[:, 0:1])
        for b in bA + bYB + bPL:
            self.P.scratch_live.append(b)
        self.scr_off = keep
        M, bM = self.carve("M", [8, 512], BF16, nbuf=8)
        SG, bSG = self.carve("SG", [3, 512], F32, nbuf=3)
        T1, bT1 = self.carve("T1", [512], F32)
        T2, bT2 = self.carve("T2", [512], F32)
        for d in range(8):
            Wa, bWa = self.load_ws(self.wcols(self.w_a_out[l], d * 128, 128))
            ps_a, bps_a = self.ps_next()
            self.mm(ps_a[:], [(Wa[:, k, :], A[:, k, :]) for k in range(8)], R=[bWa] + bA, bps=bps_a)
            Wb, bWb = self.load_ws(self.wcols(self.w_b_out[l], d * 128, 128))
            ps_b, bps_b = self.ps_next()
            self.mm(ps_b[:], [(Wb[:, k, :], YB[:, k, :]) for k in range(8)], R=[bWb] + bYB, bps=bps_b)
            g, dd = d // 2, d % 2
            ps_c, bps_c = self.ps_next()
            self.mm(ps_c[:], [(self.PW[:, g, cc, dd * 128:(dd + 1) * 128], PL[:, 2 * g + cc, :]) for cc in range(2)],
                    R=[self.bPW, bPL[2 * g], bPL[2 * g + 1]], bps=bps_c)
            gates = []
            for br in range(3):
                ps_x, bps_x = inproj(48 + 8 * br + d)
                self.act(lambda e, ps_x=ps_x, br=br, d=d: e.activation(out=SG[:, br, :], in_=ps_x[:], func=AF.Sigmoid,
                                                                       bias=bias(O_BIN, 48 + 8 * br + d), scale=1.0),
                         R=[bps_x, self.bPFM], W=[bSG[br]])
            self.dve(lambda e, ps_a=ps_a, d=d: e.scalar_tensor_tensor(T1[:], ps_a[:], bias(O_BAO, d), SG[:, 0, :],
                                                                      ALU.add, ALU.mult),
                     R=[bps_a, bSG[0], self.bPFM], W=[bT1])
            self.dve(lambda e, ps_b=ps_b: e.tensor_tensor(T2[:], ps_b[:], SG[:, 1, :], ALU.mult), R=[bps_b, bSG[1]], W=[bT2])
            self.dve(lambda e: e.tensor_tensor(T1[:], T1[:], T2[:], ALU.add), R=[bT1, bT2], W=[bT1])
            self.dve(lambda e, ps_c=ps_c, d=d: e.scalar_tensor_tensor(T2[:], ps_c[:], bias(O_PSC, d), SG[:, 2, :],
                                                                      ALU.mult, ALU.mult),
                     R=[bps_c, bSG[2], self.bPFM], W=[bT2])
            self.dve(lambda e, d=d: e.tensor_tensor(M[:, d, :], T1[:], T2[:], ALU.add), R=[bT1, bT2], W=[bM[d]])

        if tt == 0:
            self.load_gb(self.rows[l, 0], self.rows[l, 1])
        for half in range(2):
            Wm, bWm = self.load_wl(self.wcols(self.w_mix[l], half * 512, 512))
            for s in range(4):
                ps, bps = self.ps_next()
                self.mm(ps[:], [(M[:, k, s * 128:(s + 1) * 128], Wm[:, k, :]) for k in range(8)], R=[bWm] + bM, bps=bps)
                self.resid(4 * tt + s, half, ps[:], bps)
        for s in range(4):
            self.ln_x(4 * tt + s)

    def attn_kv(self, l):
        self.new_phase()
        self.KT, self.bKT = self.carve("KT", [8, MEM], BF16)
        self.V, self.bV = self.carve("V", [2, D], BF16)
        self.attn_keep = self.scr_off
        KT, bKT, V, bV = self.KT, self.bKT, self.V, self.bV
        for f in range(8):
            W, bW = self.load_ws(self.wcols(self.w_xk[l], f * 128, 128))
            ps, bps = self.ps_next()
            self.mm(ps[:, 0:MEM], [(W[:, k, :], self.MEMT[:, k, :]) for k in range(8)], R=[bW] + self.bMEMT, bps=bps)
            self.act(lambda e, ps=ps, f=f: e.activation(out=KT[:, f, :], in_=ps[:, 0:MEM], func=AF.Copy), R=[bps], W=[bKT])
        for half in range(2):
            W, bW = self.load_wl(self.wcols(self.w_xv[l], half * 512, 512))
            for mc in range(2):
                ps, bps = self.ps_next()
                self.mm(ps[:], [(self.MEMT[:, k, mc * 128:(mc + 1) * 128], W[:, k, :]) for k in range(8)],
                        R=[bW] + self.bMEMT, bps=bps)
                self.act(lambda e, ps=ps, mc=mc, half=half: e.activation(out=V[:, mc, half * 512:(half + 1) * 512],
                                                                         in_=ps[:], func=AF.Copy), R=[bps], W=[bV])
        self.load_gb(self.rows[l, 2], self.rows[l, 3])

    def attn_tile(self, l, tt):
        P = self.P
        KT, bKT, V, bV = self.KT, self.bKT, self.V, self.bV
        xt_bufs = [self.bXT[4 * tt + s] for s in range(4)]
        tsl = slice(tt * 512, (tt + 1) * 512)
        self.P.phase_barrier(self.DUMMY[:, 0:1])
        self.P.scratch_live.extend([bKT, bV])
        self.scr_off = self.attn_keep
        QT, bQT = self.carve("QT", [8, 512], BF16, nbuf=8)
        OT, bOT = self.carve("OT", [8, 512], BF16, nbuf=8)
        PT, bPT = self.carve("PT", [2, 2, 512], BF16, nbuf=2)
        PF, bPF = self.carve("PF", [2, MEM], F32, nbuf=2)
        PN, bPN = self.carve("PN", [2, MEM], BF16, nbuf=2)
        SM, bSM = self.carve("SM", [4, 4], F32, nbuf=4)
        for f in range(8):
            W, bW = self.load_ws(self.wcols(self.w_xq[l], f * 128, 128))
            ps, bps = self.ps_next()
            self.mm(ps[:], [(W[:, k, :], self.XT[:, k, tsl]) for k in range(8)], R=[bW] + xt_bufs, bps=bps)
            self.act(lambda e, ps=ps, f=f: e.activation(out=QT[:, f, :], in_=ps[:], func=AF.Copy), R=[bps], W=[bQT[f]])
        it = 0
        for h in range(4):
            pt = h % 2
            for s in range(4):
                r = it % 2
                q = it % 4
                it += 1
                ps, bps = self.ps_next()
                self.mm(ps[:, 0:MEM], [(QT[:, 2 * h + kk, s * 128:(s + 1) * 128], KT[:, 2 * h + kk, :]) for kk in range(2)],
                        R=[bQT[2 * h], bQT[2 * h + 1], bKT], bps=bps)
                self.dve(lambda e, ps=ps, q=q: e.reduce_max(SM[:, q, 0:1], ps[:, 0:MEM], AX.X), R=[bps], W=[bSM[q]])
                self.dve(lambda e, q=q: e.tensor_scalar(SM[:, q, 1:2], SM[:, q, 0:1], -1.0 / 16.0, None, ALU.mult),
                         R=[bSM[q]], W=[bSM[q]])
                self.act(lambda e, ps=ps, r=r, q=q: e.activation(out=PF[:, r, :], in_=ps[:, 0:MEM], func=AF.Exp,
                                                                 bias=SM[:, q, 1:2], scale=1.0 / 16.0),
                         R=[bps, bSM[q]], W=[bPF[r]])
                self.dve(lambda e, r=r, q=q: e.reduce_sum(SM[:, q, 2:3], PF[:, r, :], AX.X), R=[bPF[r]], W=[bSM[q]])
                self.dve(lambda e, q=q: e.reciprocal(SM[:, q, 3:4], SM[:, q, 2:3]), R=[bSM[q]], W=[bSM[q]])
                self.dve(lambda e, r=r, q=q: e.tensor_scalar(PN[:, r, :], PF[:, r, :], SM[:, q, 3:4], None, ALU.mult),
                         R=[bPF[r], bSM[q]], W=[bPN[r]])
                ps2, bps2 = self.ps_next()
                ps2b = ps2[:].bitcast(BF16)

                def tr(e, r=r, ps2b=ps2b):
                    for mc in range(2):
                        ins = e.transpose(ps2b[:, mc * 128:(mc + 1) * 128], PN[:, r, mc * 128:(mc + 1) * 128], self.IDENT[:])
                    return ins

                P.op("pe", tr, R=[bPN[r], self.bID], W=[bps2])
                self.act(lambda e, ps2b=ps2b, pt=pt, s=s: e.activation(
                    out=PT[:, pt, :, s * 128:(s + 1) * 128], in_=ps2b[:, 0:256].rearrange("p (m t) -> p m t", m=2),
                    func=AF.Copy), R=[bps2], W=[bPT[pt]])
            for kk in range(2):
                ps, bps = self.ps_next()
                self.mm(ps[:], [(V[:, mc, (2 * h + kk) * 128:(2 * h + kk + 1) * 128], PT[:, pt, mc, :]) for mc in range(2)],
                        R=[bV, bPT[pt]], bps=bps)
                self.act(lambda e, ps=ps, h=h, kk=kk: e.activation(out=OT[:, 2 * h + kk, :], in_=ps[:], func=AF.Copy),
                         R=[bps], W=[bOT[2 * h + kk]])
        for half in range(2):
            Wo, bWo = self.load_wl(self.wcols(self.w_xo[l], half * 512, 512))
            for s in range(4):
                ps, bps = self.ps_next()
                self.mm(ps[:], [(OT[:, k, s * 128:(s + 1) * 128], Wo[:, k, :]) for k in range(8)], R=[bWo] + bOT, bps=bps)
                self.resid(4 * tt + s, half, ps[:], bps)
        for s in range(4):
            self.ln_x(4 * tt + s)

    def moe(self, l):
        P = self.P
        PFM = self.PFM
        self.new_phase()
        HT, bHT = self.carve("HT", [8, T], BF16, nbuf=8)
        G2, bG2 = self.carve("G2", [16, NE], F32, nbuf=16)
        GT, bGT = self.carve("GT", [T], BF16, nbuf=16)
        LG, bLG = self.carve("LG", [2, NE], F32, nbuf=2)
        EX, bEX = self.carve("EX", [2, NE], F32, nbuf=2)
        M8, bM8 = self.carve("M8", [2, 12], F32, nbuf=2)
        GBF, bGBF = self.carve("GBF", [2, NE], BF16, nbuf=2)
        GCt, bGCt = self.carve("GCt", [2, 512], F32, nbuf=2)
        UCt, bUCt = self.carve("UCt", [2, 512], F32, nbuf=2)
        TS, bTS, RL, bRL = GCt, bGCt, UCt, bUCt
        for i in range(16):
            r = i % 2
            ps, bps = self.ps_next()
            self.mm(ps[:, 0:NE], [(self.XT[:, k, i * 128:(i + 1) * 128], self.RW[:, k, :]) for k in range(8)],
                    R=[self.bXT[i], self.bRW], bps=bps)
            self.dve(lambda e, ps=ps, r=r: e.tensor_tensor(LG[:, r, :], ps[:, 0:NE], self.RB[:], ALU.add),
                     R=[bps, self.bRB], W=[bLG[r]])
            self.dve(lambda e, r=r: e.max(M8[:, r, 0:8], LG[:, r, :]), R=[bLG[r]], W=[bM8[r]])
            self.dve(lambda e, r=r: e.tensor_scalar(M8[:, r, 8:9], M8[:, r, 0:1], -1.0, None, ALU.mult), R=[bM8[r]], W=[bM8[r]])
            self.act(lambda e, r=r: e.activation(out=EX[:, r, :], in_=LG[:, r, :], func=AF.Exp, bias=M8[:, r, 8:9], scale=1.0),
                     R=[bLG[r], bM8[r]], W=[bEX[r]])
            self.dve(lambda e, r=r: e.tensor_scalar(LG[:, r, :], LG[:, r, :], M8[:, r, 3:4], None, ALU.is_ge),
                     R=[bLG[r], bM8[r]], W=[bLG[r]])
            self.dve(lambda e, r=r: e.tensor_tensor(EX[:, r, :], EX[:, r, :], LG[:, r, :], ALU.mult), R=[bEX[r], bLG[r]], W=[bEX[r]])
            self.dve(lambda e, r=r: e.reduce_sum(M8[:, r, 9:10], EX[:, r, :], AX.X), R=[bEX[r]], W=[bM8[r]])
            self.dve(lambda e, r=r: e.reciprocal(M8[:, r, 10:11], M8[:, r, 9:10]), R=[bM8[r]], W=[bM8[r]])
            self.dve(lambda e, r=r: e.tensor_scalar(GBF[:, r, :], EX[:, r, :], M8[:, r, 10:11], None, ALU.mult),
                     R=[bEX[r], bM8[r]], W=[bGBF[r]])
            self.dve(lambda e, r=r, i=i: e.tensor_scalar(G2[:, i, :], EX[:, r, :], M8[:, r, 10:11], 1.0 / 1.702,
                                                         ALU.mult, ALU.mult), R=[bEX[r], bM8[r]], W=[bG2[i]])
            ps2, bps2 = self.ps_next()
            ps2b = ps2[:].bitcast(BF16)
            P.op("pe", lambda e, r=r, ps2b=ps2b: e.transpose(ps2b[0:NE, 0:128], GBF[:, r, :], self.IDENT[:]),
                 R=[bGBF[r], self.bID], W=[bps2])
            self.act(lambda e, ps2b=ps2b, i=i: e.activation(out=GT[0:NE, i * 128:(i + 1) * 128], in_=ps2b[0:NE, 0:128],
                                                            func=AF.Copy), R=[bps2], W=[bGT[i]])
        for i in range(16):
            for half in range(2):
                ps, bps = self.ps_next()
                self.mm(ps[:], [(GT[0:NE, i * 128:(i + 1) * 128], self.BD[:, half * 512:(half + 1) * 512])],
                        R=[bGT[i], self.bBD], bps=bps)
                self.resid(i, half, ps[:], bps)
        it = 0
        for ex in range(self.ne):
            wgu = self.w_gu[l, ex]
            for j in range(8):
                Wg, bWg = self.load_ws(self.wcols(wgu, j * 128, 128))
                Wu, bWu = self.load_ws(self.wcols(wgu, D + j * 128, 128))
                bg = PFM[:, O_BGU + ex * 16 + j:O_BGU + ex * 16 + j + 1]
                bu = PFM[:, O_BGU + ex * 16 + 8 + j:O_BGU + ex * 16 + 8 + j + 1]
                for tt in range(4):
                    r = it % 2
                    it += 1
                    tsl = slice(tt * 512, (tt + 1) * 512)
                    xt_bufs = [self.bXT[4 * tt + s] for s in range(4)]
                    psg, bpsg = self.ps_next()
                    self.mm(psg[:], [(Wg[:, k, :], self.XT[:, k, tsl]) for k in range(8)], R=[bWg] + xt_bufs, bps=bpsg)
                    psu, bpsu = self.ps_next()
                    self.mm(psu[:], [(Wu[:, k, :], self.XT[:, k, tsl]) for k in range(8)], R=[bWu] + xt_bufs, bps=bpsu)
                    self.dve(lambda e, psg=psg, bg=bg, r=r: e.tensor_scalar(GCt[:, r, :], psg[:], bg, 7.0, ALU.add, ALU.min),
                             R=[bpsg, self.bPFM], W=[bGCt[r]])
                    self.act(lambda e, r=r: e.activation(out=TS[:, r, :], in_=GCt[:, r, :], func=AF.Silu, scale=1.702),
                             R=[bGCt[r]], W=[bTS[r]])
                    self.dve(lambda e, psu=psu, bu=bu, r=r: e.tensor_scalar(UCt[:, r, :], psu[:], bu, 7.0, ALU.add, ALU.min),
                             R=[bpsu, self.bPFM], W=[bUCt[r]])
                    self.act(lambda e, r=r: e.activation(out=RL[:, r, :], in_=UCt[:, r, :], func=AF.Relu, bias=7.0, scale=1.0),
                             R=[bUCt[r]], W=[bRL[r]])
                    self.dve(lambda e, r=r, j=j, tsl=tsl: e.scalar_tensor_tensor(HT[:, j, tsl], RL[:, r, :], -6.0, TS[:, r, :],
                                                                                 ALU.add, ALU.mult),
                             R=[bRL[r], bTS[r]], W=[bHT[j]])
            for half in range(2):
                Wd, bWd = self.load_wl(self.w_down[l, ex].rearrange("(kc p) f -> p kc f", p=128)[:, :, half * 512:(half + 1) * 512])
                for i in range(16):
                    ps, bps = self.ps_next()
                    self.mm(ps[:], [(HT[:, k, i * 128:(i + 1) * 128], Wd[:, k, :]) for k in range(8)], R=[bWd] + bHT, bps=bps)
                    xs = self.X[:, i, half * 512:(half + 1) * 512]
                    self.dve(lambda e, ps=ps, xs=xs, i=i, ex=ex: e.scalar_tensor_tensor(
                        xs, ps[:], G2[:, i, ex:ex + 1], xs, ALU.mult, ALU.add),
                        R=[bps, bG2[i], self.bX[i]], W=[self.bX[i]])
        self.load_gb(self.rows[l, 4], self.rows[l, 5])
        for i in range(16):
            self.ln_x(i)

    def build(self):
        nc = self.nc
        with nc.Fori(0, self.NS) as s:
            self.setup_consts()
            self.load_seq(s)
            for l in range(self.L):
                self.load_layer_params(l)
                if self.do_mixer:
                    for tt in range(4):
                        self.mixer_tile(l, tt)
                if self.do_attn:
                    self.attn_kv(l)
                    for tt in range(4):
                        self.attn_tile(l, tt)
                if self.do_moe:
                    self.moe(l)
            self.store_seq(s)
            sems = self.P.emit(self.bX)
            nc.all_engine_barrier()
            nc.gpsimd.dma_reset()
            for sm in sems:
                nc.sync.sem_clear(sm)
            nc.all_engine_barrier()
        return nc


def _pack_pfm(inp, l):
    def fm(v):
        return np.ascontiguousarray(v.reshape(-1, 128).T)

    cols = [fm(inp["b_in"][l]),
            np.ascontiguousarray(inp["conv_a_w"][l].T.reshape(8, 128, 31).transpose(1, 0, 2).reshape(128, 248)),
            fm(inp["conv_a_b"][l]), fm(inp["ln_a_g"][l]), fm(inp["ln_a_b"][l]), fm(inp["b_a_out"][l]),
            np.ascontiguousarray(inp["conv_b_w"][l].T.reshape(8, 128, 3).transpose(1, 0, 2).reshape(128, 24)),
            fm(inp["pool_scale"][l]),
            np.ascontiguousarray(inp["b_gu"][l].reshape(NE, 16, 128).transpose(2, 0, 1).reshape(128, NE * 16))]
    out = np.concatenate(cols, axis=1).astype(np.float32)
    assert out.shape == (128, NPF)
    return out


def make_inmaps(inp, layers, n_cores, nseq, x_override=None):
    ls = list(layers)
    f32 = lambda a: np.ascontiguousarray(np.asarray(a, dtype=np.float32))
    shared = {
        "memln": f32(np.stack([inp["mem_ln_g"], inp["mem_ln_b"]])),
        "ident": np.eye(128, dtype=np.float32),
        "pfm": f32(np.stack([_pack_pfm(inp, l) for l in ls])),
        "rows": f32(np.stack([np.stack([inp["ln1_g"][l], inp["ln1_b"][l], inp["ln2_g"][l], inp["ln2_b"][l],
                                        inp["ln3_g"][l], inp["ln3_b"][l]]) for l in ls])),
    }
    for k in ("router_b", "w_in", "w_a_out", "w_b_out", "pool_w", "w_mix_out", "w_xq", "w_xk", "w_xv", "w_xo",
              "router_w", "w_gu", "w_down", "b_down"):
        a = inp[k]
        shared[k] = f32(a[ls[0]:ls[-1] + 1]) if ls == list(range(ls[0], ls[-1] + 1)) else f32(a[ls])
    x = inp["x"] if x_override is None else x_override
    maps = []
    for c in range(n_cores):
        m = dict(shared)
        m["x"] = f32(x[c * nseq:(c + 1) * nseq])
        m["mem"] = f32(inp["mem"][c * nseq:(c + 1) * nseq])
        maps.append(m)
    return maps


_NC_CACHE = {}


def _program(L, NS, **kw):
    key = (L, NS, tuple(sorted(kw.items())))
    if key not in _NC_CACHE:
        _NC_CACHE[key] = Builder(L, NS, **kw).build()
    return _NC_CACHE[key]


FUSED = True


def kernel(**inputs):
    inp = {k: np.asarray(v) for k, v in inputs.items()}
    if FUSED:
        nc = _program(DEPTH, NSEQ)
        maps = make_inmaps(inp, range(DEPTH), NCORES, NSEQ)
        res = run_bass_kernel_spmd(nc, maps, core_ids=list(range(NCORES)))
        return np.concatenate([r["y"] for r in res.results], axis=0).astype(np.float32)
    x = inp["x"]
    nc = _program(1, NSEQ)
    for l in range(DEPTH):
        maps = make_inmaps(inp, [l], NCORES, NSEQ, x_override=x)
        res = run_bass_kernel_spmd(nc, maps, core_ids=list(range(NCORES)))
        x = np.concatenate([r["y"] for r in res.results], axis=0).astype(np.float32)
    return x
```

```python
import numpy as np
import concourse.bass as bass
import concourse.mybir as mybir
from concourse.bass_utils import run_bass_kernel_spmd

F32 = mybir.dt.float32
BF16 = mybir.dt.bfloat16
AF = mybir.ActivationFunctionType
ALU = mybir.AluOpType
AX = mybir.AxisListType

D = 1024
T = 2048
MEM = 256
NE = 32
FIN = 9216
DEPTH = 4
ALPHA = float((2 * DEPTH) ** 0.25)
EPS = 1e-5
NCORES = 8
NSEQ = 4

O_BIN, O_CAW, O_CAB, O_LAG, O_LAB, O_BAO, O_CBW, O_PSC, O_BGU = 0, 72, 320, 328, 336, 344, 352, 376, 384
NPF = 896

EPOCH = 24000
ENGS = ("pe", "act", "dve", "pool", "sp")


class Buf:
    __slots__ = ("name", "writer", "readers", "dma_readers", "sem", "ndma")

    def __init__(self, name, writer=None):
        self.name = name
        self.writer = writer
        self.readers = {}
        self.dma_readers = []
        self.sem = None
        self.ndma = 0


class Op:
    __slots__ = ("eng", "fn", "deps", "is_dma", "buf", "dma_val", "needs_inc", "ev", "waits", "seq")

    def __init__(self, eng, fn):
        self.eng = eng
        self.fn = fn
        self.deps = []
        self.is_dma = False
        self.buf = None
        self.dma_val = 0
        self.needs_inc = False
        self.ev = None
        self.waits = None
        self.seq = 0


class Prog:
    def __init__(self, nc):
        self.nc = nc
        self.streams = {e: [] for e in ENGS}
        self.nops = 0
        self.bufs = []
        self.scratch_live = []
        self.barrier_op = None

    def buf(self, name, scratch=False):
        b = Buf(name, self.barrier_op if scratch else None)
        self.bufs.append(b)
        if scratch:
            self.scratch_live.append(b)
        return b

    def op(self, eng, fn, R=(), W=(), dma=None, ndma=1):
        o = Op(eng, fn)
        self.nops += 1
        o.seq = self.nops
        deps = {}

        def add(d):
            if d is None or d is o:
                return
            if d.is_dma:
                deps[id(d)] = d
                return
            if d.eng == "pe" and eng == "pe":
                return
            k = d.eng
            if k not in deps or deps[k].seq < d.seq:
                deps[k] = d

        for b in R:
            add(b.writer)
        for b in W:
            add(b.writer)
            for r in b.readers.values():
                add(r)
            for r in b.dma_readers:
                add(r)
        o.deps = list(deps.values())
        for d in o.deps:
            if not d.is_dma:
                d.needs_inc = True
        if dma is not None:
            o.is_dma = True
            o.buf = dma
            dma.ndma += ndma
            o.dma_val = 16 * dma.ndma
        for b in W:
            b.writer = o
            b.readers = {}
            b.dma_readers = []
        for b in R:
            if b.writer is o:
                continue
            if o.is_dma:
                b.dma_readers.append(o)
            else:
                b.readers[eng] = o
        self.streams[eng].append(o)
        return o

    def phase_barrier(self, dummy_ap):
        live = self.scratch_live
        self.scratch_live = []
        tok = Buf("phase_tok")
        o = self.op("dve", lambda e: e.memset(dummy_ap, 0.0), R=(), W=live + [tok])
        o.needs_inc = True
        self.barrier_op = o
        return o

    def emit(self, final_bufs):
        nc = self.nc
        sems = {}

        def get_sem(key):
            if key not in sems:
                sems[key] = nc.alloc_semaphore("s_%s_%d" % key)
            return sems[key]

        for e in ENGS:
            cnt = 0
            for o in self.streams[e]:
                if o.is_dma:
                    if o.buf.sem is None:
                        o.buf.sem = nc.alloc_semaphore("d%d_%s" % (len(sems) + o.seq, o.buf.name))
                    continue
                if o.needs_inc:
                    ep, v = divmod(cnt, EPOCH)
                    o.ev = (e, ep, v + 1)
                    cnt += 1
        for e in ENGS:
            seen_c = {}
            seen_d = {}
            for o in self.streams[e]:
                w = []
                for d in o.deps:
                    if d.is_dma:
                        s = d.buf.sem
                        if seen_d.get(id(s), 0) < d.dma_val:
                            seen_d[id(s)] = d.dma_val
                            w.append((s, d.dma_val))
                    else:
                        pe, ep, v = d.ev
                        if seen_c.get(pe, (-1, 0)) < (ep, v):
                            seen_c[pe] = (ep, v)
                            w.append((get_sem((pe, ep)), v))
                o.waits = w
        engobj = {"pe": nc.tensor, "act": nc.scalar, "dve": nc.vector, "pool": nc.gpsimd, "sp": nc.sync}
        for e in ENGS:
            eng = engobj[e]
            for o in self.streams[e]:
                for (s, v) in o.waits:
                    eng.wait_ge(s, v)
                r = o.fn(eng)
                if o.is_dma:
                    if not isinstance(r, (list, tuple)):
                        r = [r]
                    for ins in r:
                        ins.then_inc(o.buf.sem, 16)
                elif o.needs_inc:
                    pe, ep, v = o.ev
                    r.then_inc(get_sem((pe, ep)), 1)
        dma_sems = [b.sem for b in self.bufs if b.sem is not None]
        for b in self.bufs:
            if b.sem is not None:
                nc.sync.wait_ge(b.sem, 16 * b.ndma)
        return list(sems.values()) + dma_sems


class Builder:
    def __init__(self, L, NS, do_mixer=True, do_attn=True, do_moe=True, ne=NE):
        self.L, self.NS = L, NS
        self.do_mixer, self.do_attn, self.do_moe, self.ne = do_mixer, do_attn, do_moe, ne
        nc = self.nc = bass.Bass("TRN2", target_bir_lowering=False)
        P = self.P = Prog(nc)

        def din(name, shape):
            return nc.dram_tensor(name, list(shape), F32, kind="ExternalInput").ap()

        self.x = din("x", [NS, T, D])
        self.mem = din("mem", [NS, MEM, D])
        self.memln = din("memln", [2, D])
        self.ident_d = din("ident", [128, 128])
        self.pfm = din("pfm", [L, 128, NPF])
        self.rows = din("rows", [L, 6, D])
        self.router_b = din("router_b", [L, NE])
        self.w_in = din("w_in", [L, D, FIN])
        self.w_a_out = din("w_a_out", [L, D, D])
        self.w_b_out = din("w_b_out", [L, D, D])
        self.pool_w = din("pool_w", [L, 4, 256, 256])
        self.w_mix = din("w_mix_out", [L, D, D])
        self.w_xq = din("w_xq", [L, D, D])
        self.w_xk = din("w_xk", [L, D, D])
        self.w_xv = din("w_xv", [L, D, D])
        self.w_xo = din("w_xo", [L, D, D])
        self.router_w = din("router_w", [L, D, NE])
        self.w_gu = din("w_gu", [L, NE, D, 2 * D])
        self.w_down = din("w_down", [L, NE, D, D])
        self.b_down = din("b_down", [L, NE, D])
        self.y = nc.dram_tensor("y", [NS, T, D], F32, kind="ExternalOutput").ap()

        def sb(name, shape, dt):
            return nc.alloc_sbuf_tensor(name, list(shape), dt)

        self.X = sb("X", [128, 16, D], F32)
        self.bX = [P.buf("X%d" % i) for i in range(16)]
        self.XT = sb("XT", [128, 8, T], BF16)
        self.bXT = [P.buf("XT%d" % i) for i in range(16)]
        self.MEMT = sb("MEMT", [128, 8, MEM], BF16)
        self.bMEMT = [P.buf("MEMT%d" % i) for i in range(2)]
        self.GB = sb("GB", [128, 2, D], F32)
        self.bGB = P.buf("GB")
        self.XB = [sb("XB%d" % i, [128, D], BF16) for i in range(2)]
        self.bXB = [P.buf("XB%d" % i) for i in range(2)]
        self.xb_i = 0
        self.IDF = sb("IDF", [128, 128], F32)
        self.IDENT = sb("IDENT", [128, 128], BF16)
        self.bID = P.buf("ident")
        self.ONES = sb("ONES", [128, 128], BF16)
        self.bONES = P.buf("ones")
        self.RC15 = sb("RC15", [128, 16], F32)
        self.bRC = P.buf("rc15")
        self.EPSC = sb("EPSC", [128, 1], F32)
        self.PFM = sb("PFM", [128, NPF], F32)
        self.bPFM = P.buf("pfm")
        self.PW = sb("PW", [128, 4, 2, 256], BF16)
        self.bPW = P.buf("pw")
        self.RW = sb("RW", [128, 8, NE], BF16)
        self.bRW = P.buf("rw")
        self.RB = sb("RB", [128, NE], F32)
        self.bRB = P.buf("rb")
        self.BD = sb("BD", [NE, D], BF16)
        self.bBD = P.buf("bd")
        self.HA = sb("HA", [128, 8, 30], F32)
        self.HBv = sb("HBv", [128, 8, 2], F32)
        self.HC = sb("HC", [128, 8, 16], F32)
        self.bHA = [P.buf("HA%d" % c) for c in range(8)]
        self.bHB = [P.buf("HB%d" % c) for c in range(8)]
        self.bHC = [P.buf("HC%d" % c) for c in range(8)]
        self.DUMMY = sb("DUMMY", [128, 8], F32)
        self.NST = 4
        self.BNS = [sb("BNS%d" % i, [128, 2, 6], F32) for i in range(self.NST)]
        self.MV = [sb("MV%d" % i, [128, 4], F32) for i in range(self.NST)]
        self.bST = [P.buf("ST%d" % i) for i in range(self.NST)]
        self.st_i = 0
        self.NWS = 6
        self.WS = [sb("WS%d" % i, [128, 8, 128], BF16) for i in range(self.NWS)]
        self.bWS = [P.buf("WS%d" % i) for i in range(self.NWS)]
        self.ws_i = 0
        self.NWL = 2
        self.WL = [sb("WL%d" % i, [128, 8, 512], BF16) for i in range(self.NWL)]
        self.bWL = [P.buf("WL%d" % i) for i in range(self.NWL)]
        self.wl_i = 0
        self.PS = [nc.alloc_psum_tensor("ps%d" % i, [128, 512], F32) for i in range(8)]
        self.bPS = [P.buf("ps%d" % i) for i in range(8)]
        self.ps_i = 0
        self.ps_n = 8
        self.SCR_BYTES = (nc.sbuf_bytes_remaining - 1024) // 64 * 64
        assert self.SCR_BYTES >= 52 * 1024, self.SCR_BYTES
        self.SCR = sb("SCR", [128, self.SCR_BYTES // 2], BF16)
        self.scr_off = 0

    def carve(self, name, free_shape, dt, nbuf=1):
        n = int(np.prod(free_shape))
        nbytes = n * (4 if dt == F32 else 2)
        nbytes_al = (nbytes + 31) // 32 * 32
        assert self.scr_off + nbytes_al <= self.SCR_BYTES, (name, self.scr_off, nbytes_al)
        ap = self.SCR[:, self.scr_off // 2:(self.scr_off + nbytes) // 2]
        self.scr_off += nbytes_al
        if dt == F32:
            ap = ap.bitcast(F32)
        if len(free_shape) == 2:
            ap = ap.rearrange("p (a b) -> p a b", a=free_shape[0])
        elif len(free_shape) == 3:
            ap = ap.rearrange("p (a b c) -> p a b c", a=free_shape[0], b=free_shape[1])
        bufs = [self.P.buf(name + str(i), scratch=True) for i in range(nbuf)]
        return ap, (bufs[0] if nbuf == 1 else bufs)

    def new_phase(self):
        self.P.phase_barrier(self.DUMMY[:, 0:1])
        self.scr_off = 0

    def ps_next(self):
        i = self.ps_i % self.ps_n
        self.ps_i += 1
        return self.PS[i], self.bPS[i]

    def wcols(self, w2d, c0, n):
        return w2d.rearrange("(kc p) f -> p kc f", p=128)[:, :, c0:c0 + n]

    def load_ws(self, src):
        s = self.ws_i % self.NWS
        self.ws_i += 1
        W, b = self.WS[s], self.bWS[s]
        self.P.op("pool", lambda e: e.dma_start(out=W[:], in_=src), W=[b], dma=b)
        return W, b

    def load_wl(self, src):
        s = self.wl_i % self.NWL
        self.wl_i += 1
        W, b = self.WL[s], self.bWL[s]
        self.P.op("pool", lambda e: e.dma_start(out=W[:], in_=src), W=[b], dma=b)
        return W, b

    def mm(self, out_ap, pairs, R, bps):
        pairs = list(pairs)

        def fn(e):
            n = len(pairs)
            for i, (l, r) in enumerate(pairs):
                ins = e.matmul(out_ap, l, r, start=(i == 0), stop=(i == n - 1))
            return ins

        self.P.op("pe", fn, R=R, W=[bps])

    def dve(self, fn, R, W):
        return self.P.op("dve", fn, R=R, W=W)

    def act(self, fn, R, W):
        return self.P.op("act", fn, R=R, W=W)

    def setup_consts(self):
        P = self.P
        IDF, IDENT, ONES, RC = self.IDF, self.IDENT, self.ONES, self.RC15
        P.op("sp", lambda e: e.dma_start(out=IDF[:], in_=self.ident_d), W=[self.bID], dma=self.bID)
        self.dve(lambda e: e.tensor_copy(IDENT[:], IDF[:]), R=[self.bID], W=[self.bID])
        self.dve(lambda e: e.memset(ONES[:], 1.0), R=[], W=[self.bONES])
        for t in range(16):
            self.dve(lambda e, t=t: e.memset(RC[:, t:t + 1], 1.0 / (t + 1)), R=[], W=[self.bRC])
        self.dve(lambda e: e.memset(self.EPSC[:], EPS), R=[], W=[self.bRC])

    def load_layer_params(self, l):
        P = self.P
        P.op("sp", lambda e: e.dma_start(out=self.PFM[:], in_=self.pfm[l]), W=[self.bPFM], dma=self.bPFM)
        P.op("sp", lambda e: e.dma_start(out=self.RB[:], in_=self.router_b[l].partition_broadcast(128)),
             W=[self.bRB], dma=self.bRB)
        pw_src = self.pool_w[l].rearrange("g (cc p) d -> p g cc d", p=128)

        def ld_pw(e):
            return [e.dma_start(out=self.PW[:, g], in_=pw_src[:, g]) for g in range(4)]

        P.op("pool", ld_pw, W=[self.bPW], dma=self.bPW, ndma=4)
        P.op("pool", lambda e: e.dma_start(out=self.RW[:], in_=self.wcols(self.router_w[l], 0, NE)),
             W=[self.bRW], dma=self.bRW)
        P.op("pool", lambda e: e.dma_start(out=self.BD[:], in_=self.b_down[l]), W=[self.bBD], dma=self.bBD)

    def load_gb(self, g_row, b_row):
        def f(e):
            return [e.dma_start(out=self.GB[:, 0, :], in_=g_row.partition_broadcast(128)),
                    e.dma_start(out=self.GB[:, 1, :], in_=b_row.partition_broadcast(128))]

        self.P.op("sp", f, W=[self.bGB], dma=self.bGB, ndma=2)

    def to_T(self, row_ap, brow, dstT, bdst):
        s = self.xb_i % 2
        self.xb_i += 1
        XB, bXB = self.XB[s], self.bXB[s]
        self.act(lambda e: e.activation(out=XB[:], in_=row_ap, func=AF.Copy), R=[brow], W=[bXB])
        ps, bps = self.ps_next()
        psb = ps[:].bitcast(BF16)

        def tr(e):
            for c in range(8):
                ins = e.transpose(psb[:, c * 128:(c + 1) * 128], XB[:, c * 128:(c + 1) * 128], self.IDENT[:])
            return ins

        self.P.op("pe", tr, R=[bXB, self.bID], W=[bps])
        self.act(lambda e: e.activation(out=dstT, in_=psb.rearrange("p (c t) -> p c t", c=8), func=AF.Copy),
                 R=[bps], W=[bdst])

    def ln_rows(self, row_ap, brow, dstT, bdst):
        k = self.st_i % self.NST
        self.st_i += 1
        BNS, MV, bST = self.BNS[k], self.MV[k], self.bST[k]
        self.dve(lambda e: e.bn_stats(BNS[:, 0, :], row_ap[:, 0:512]), R=[brow], W=[bST])
        self.dve(lambda e: e.bn_stats(BNS[:, 1, :], row_ap[:, 512:1024]), R=[brow], W=[bST])
        self.dve(lambda e: e.bn_aggr(MV[:, 0:2], BNS[:].rearrange("p a b -> p (a b)")), R=[bST], W=[bST])
        self.act(lambda e: e.activation(out=MV[:, 2:3], in_=MV[:, 1:2], func=AF.Sqrt, bias=self.EPSC[:, 0:1], scale=1.0),
                 R=[bST, self.bRC], W=[bST])
        self.dve(lambda e: e.reciprocal(MV[:, 2:3], MV[:, 2:3]), R=[bST], W=[bST])
        self.dve(lambda e: e.scalar_tensor_tensor(MV[:, 3:4], MV[:, 0:1], -1.0, MV[:, 2:3], ALU.mult, ALU.mult),
                 R=[bST], W=[bST])
        self.act(lambda e: e.activation(out=row_ap, in_=row_ap, func=AF.Identity, bias=MV[:, 3:4], scale=MV[:, 2:3]),
                 R=[brow, bST], W=[brow])
        self.dve(lambda e: e.tensor_tensor(row_ap, row_ap, self.GB[:, 0, :], ALU.mult), R=[brow, self.bGB], W=[brow])
        self.dve(lambda e: e.tensor_tensor(row_ap, row_ap, self.GB[:, 1, :], ALU.add), R=[brow, self.bGB], W=[brow])
        self.to_T(row_ap, brow, dstT, bdst)

    def ln_x(self, i):
        self.ln_rows(self.X[:, i, :], self.bX[i], self.XT[:, :, i * 128:(i + 1) * 128], self.bXT[i])

    def to_T_batch(self, items):
        for p0 in range(0, len(items), 2):
            grp = items[p0:p0 + 2]
            st = []
            for (row_ap, brow, dstT, bdst) in grp:
                s_ = self.xb_i % 2
                self.xb_i += 1
                XB, bXB = self.XB[s_], self.bXB[s_]
                self.act(lambda e, XB=XB, row_ap=row_ap: e.activation(out=XB[:], in_=row_ap, func=AF.Copy), R=[brow], W=[bXB])
                st.append((XB, bXB))
            pss = []
            for (XB, bXB) in st:
                ps, bps = self.ps_next()
                psb = ps[:].bitcast(BF16)

                def tr(e, XB=XB, psb=psb):
                    for c in range(8):
                        ins = e.transpose(psb[:, c * 128:(c + 1) * 128], XB[:, c * 128:(c + 1) * 128], self.IDENT[:])
                    return ins

                self.P.op("pe", tr, R=[bXB, self.bID], W=[bps])
                pss.append((psb, bps))
            for (psb, bps), (row_ap, brow, dstT, bdst) in zip(pss, grp):
                self.act(lambda e, psb=psb, dstT=dstT: e.activation(out=dstT, in_=psb.rearrange("p (c t) -> p c t", c=8),
                                                                    func=AF.Copy), R=[bps], W=[bdst])

    def ln_batch(self, tiles):
        assert len(tiles) <= self.NST
        ks = []
        for i in tiles:
            ks.append(self.st_i % self.NST)
            self.st_i += 1
        rows = [(self.X[:, i, :], self.bX[i]) for i in tiles]
        for h in range(2):
            for (row_ap, brow), k in zip(rows, ks):
                self.dve(lambda e, row_ap=row_ap, k=k, h=h: e.bn_stats(self.BNS[k][:, h, :], row_ap[:, h * 512:(h + 1) * 512]),
                         R=[brow], W=[self.bST[k]])
        for k in ks:
            self.dve(lambda e, k=k: e.bn_aggr(self.MV[k][:, 0:2], self.BNS[k][:].rearrange("p a b -> p (a b)")),
                     R=[self.bST[k]], W=[self.bST[k]])
        for k in ks:
            self.act(lambda e, k=k: e.activation(out=self.MV[k][:, 2:3], in_=self.MV[k][:, 1:2], func=AF.Sqrt,
                                                 bias=self.EPSC[:, 0:1], scale=1.0), R=[self.bST[k], self.bRC], W=[self.bST[k]])
        for k in ks:
            self.dve(lambda e, k=k: e.reciprocal(self.MV[k][:, 2:3], self.MV[k][:, 2:3]), R=[self.bST[k]], W=[self.bST[k]])
        for k in ks:
            self.dve(lambda e, k=k: e.scalar_tensor_tensor(self.MV[k][:, 3:4], self.MV[k][:, 0:1], -1.0, self.MV[k][:, 2:3],
                                                           ALU.mult, ALU.mult), R=[self.bST[k]], W=[self.bST[k]])
        for (row_ap, brow), k in zip(rows, ks):
            self.act(lambda e, row_ap=row_ap, k=k: e.activation(out=row_ap, in_=row_ap, func=AF.Identity,
                                                                bias=self.MV[k][:, 3:4], scale=self.MV[k][:, 2:3]),
                     R=[brow, self.bST[k]], W=[brow])
        for (row_ap, brow) in rows:
            self.dve(lambda e, row_ap=row_ap: e.tensor_tensor(row_ap, row_ap, self.GB[:, 0, :], ALU.mult),
                     R=[brow, self.bGB], W=[brow])
        for (row_ap, brow) in rows:
            self.dve(lambda e, row_ap=row_ap: e.tensor_tensor(row_ap, row_ap, self.GB[:, 1, :], ALU.add),
                     R=[brow, self.bGB], W=[brow])
        self.to_T_batch([(self.X[:, i, :], self.bX[i], self.XT[:, :, i * 128:(i + 1) * 128], self.bXT[i]) for i in tiles])

    def resid(self, i, half, ps_ap, bps):
        xs = self.X[:, i, half * 512:(half + 1) * 512]
        self.dve(lambda e: e.scalar_tensor_tensor(xs, xs, ALPHA, ps_ap, ALU.mult, ALU.add),
                 R=[self.bX[i], bps], W=[self.bX[i]])

    def load_seq(self, s):
        P = self.P
        for i in range(16):
            P.op("sp", lambda e, i=i: e.dma_start(out=self.X[:, i, :], in_=self.x[s, i * 128:(i + 1) * 128, :]),
                 W=[self.bX[i]], dma=self.bX[i])
        self.to_T_batch([(self.X[:, i, :], self.bX[i], self.XT[:, :, i * 128:(i + 1) * 128], self.bXT[i]) for i in range(16)])
        self.new_phase()
        MR, bMR = self.carve("MR", [2, D], F32, nbuf=2)
        self.load_gb(self.memln[0], self.memln[1])
        for mc in range(2):
            P.op("sp", lambda e, mc=mc: e.dma_start(out=MR[:, mc, :], in_=self.mem[s, mc * 128:(mc + 1) * 128, :]),
                 W=[bMR[mc]], dma=bMR[mc])
            self.ln_rows(MR[:, mc, :], bMR[mc], self.MEMT[:, :, mc * 128:(mc + 1) * 128], self.bMEMT[mc])

    def store_seq(self, s):
        for i in range(16):
            self.P.op("sp", lambda e, i=i: e.dma_start(out=self.y[s, i * 128:(i + 1) * 128, :], in_=self.X[:, i, :]),
                      R=[self.bX[i]], dma=self.bX[i])

    def mixer_tile(self, l, tt):
        P = self.P
        PFM = self.PFM
        w_in = self.w_in[l]
        xt_bufs = [self.bXT[4 * tt + s] for s in range(4)]
        tsl = slice(tt * 512, (tt + 1) * 512)

        def inproj(j):
            W, bW = self.load_ws(self.wcols(w_in, j * 128, 128))
            ps, bps = self.ps_next()
            self.mm(ps[:], [(W[:, k, :], self.XT[:, k, tsl]) for k in range(8)], R=[bW] + xt_bufs, bps=bps)
            return ps, bps

        def bias(off, j):
            return PFM[:, off + j:off + j + 1]

        self.new_phase()
        A, bA = self.carve("A", [8, 512], BF16, nbuf=8)
        YB, bYB = self.carve("YB", [8, 512], BF16, nbuf=8)
        PL, bPL = self.carve("PL", [8, 512], BF16, nbuf=8)
        GA, bGA = self.carve("GA", [512], F32)
        ABUF, bAB = self.carve("ABUF", [544], BF16)
        DG, bDG = self.carve("DG", [31, 128], BF16)
        GC, bGC = self.carve("GC", [512], F32)
        GV, bGV = self.carve("GV", [516], F32)
        U, bU = GA, bGA
        PIN, bPIN = self.carve("PIN", [528], F32)
        S0, bS0 = self.carve("S0", [528], F32)
        S1, bS1 = self.carve("S1", [528], F32)
        SQ, bSQ = self.carve("SQ", [2, 512], BF16, nbuf=2)
        MEANB, bMEANB = S0[:, 0:512], bS0
        RSTDB, bRSTDB = S1[:, 0:512], bS1
        XH, bXH = self.carve("XH", [512], F32)
        self.ps_n = 6
        psS1, bpsS1 = self.PS[6], self.bPS[6]
        psS2, bpsS2 = self.PS[7], self.bPS[7]

        for c in range(8):
            ps_l, bps_l = inproj(c)
            ps_g, bps_g = inproj(8 + c)
            self.act(lambda e, ps_g=ps_g, c=c: e.activation(out=GA[:], in_=ps_g[:], func=AF.Sigmoid,
                                                            bias=bias(O_BIN, 8 + c), scale=1.0),
                     R=[bps_g, self.bPFM], W=[bGA])
            if tt == 0:
                self.dve(lambda e: e.memset(ABUF[:, 0:30], 0.0), R=[], W=[bAB])
            else:
                self.dve(lambda e, c=c: e.tensor_copy(ABUF[:, 0:30], self.HA[:, c, :]), R=[self.bHA[c]], W=[bAB])
            self.dve(lambda e, ps_l=ps_l, c=c: e.scalar_tensor_tensor(ABUF[:, 30:542], ps_l[:], bias(O_BIN, c), GA[:],
                                                                      ALU.add, ALU.mult),
                     R=[bps_l, bGA, self.bPFM], W=[bAB])
            if tt < 3:
                self.act(lambda e, c=c: e.activation(out=self.HA[:, c, :], in_=ABUF[:, 512:542], func=AF.Copy),
                         R=[bAB], W=[self.bHA[c]])
            self.dve(lambda e, c=c: e.tensor_tensor(
                DG[:], self.IDENT[:].unsqueeze(1).broadcast_to([128, 31, 128]),
                PFM[:, O_CAW + c * 31:O_CAW + (c + 1) * 31].unsqueeze(2).broadcast_to([128, 31, 128]), ALU.mult),
                R=[self.bID, self.bPFM], W=[bDG])
            ps_cv, bps_cv = self.ps_next()
            self.mm(ps_cv[:], [(DG[:, k, :], ABUF[:, k:k + 512]) for k in range(31)], R=[bDG, bAB], bps=bps_cv)
            self.act(lambda e, c=c, ps_cv=ps_cv: e.activation(out=A[:, c, :], in_=ps_cv[:], func=AF.Identity,
                                                              bias=PFM[:, O_CAB + c:O_CAB + c + 1], scale=1.0),
                     R=[bps_cv, self.bPFM], W=[bA[c]])
            sq = c % 2
            self.act(lambda e, c=c, sq=sq, ps_cv=ps_cv: e.activation(out=SQ[:, sq, :], in_=ps_cv[:], func=AF.Square,
                                                                     bias=PFM[:, O_CAB + c:O_CAB + c + 1], scale=1.0),
                     R=[bps_cv, self.bPFM], W=[bSQ[sq]])

            def st1(e, c=c):
                return e.matmul(psS1[:], self.ONES[:], A[:, c, :], start=(c == 0), stop=(c == 7))

            def st2(e, c=c, sq=sq):
                return e.matmul(psS2[:], self.ONES[:], SQ[:, sq, :], start=(c == 0), stop=(c == 7))

            P.op("pe", st1, R=[bA[c], self.bONES], W=[bpsS1])
            P.op("pe", st2, R=[bSQ[sq], self.bONES], W=[bpsS2])

            ps_gc, bps_gc = inproj(24 + c)
            ps_v, bps_v = inproj(32 + c)
            ps_gb, bps_gb = inproj(16 + c)
            self.act(lambda e, ps_gc=ps_gc, c=c: e.activation(out=GC[:], in_=ps_gc[:], func=AF.Identity,
                                                              bias=bias(O_BIN, 24 + c), scale=1.0),
                     R=[bps_gc, self.bPFM], W=[bGC])
            if tt == 0:
                self.dve(lambda e: e.memset(GV[:, 0:2], 0.0), R=[], W=[bGV])
            else:
                self.dve(lambda e, c=c: e.tensor_copy(GV[:, 0:2], self.HBv[:, c, :]), R=[self.bHB[c]], W=[bGV])
            self.dve(lambda e, ps_v=ps_v, c=c: e.scalar_tensor_tensor(GV[:, 2:514], ps_v[:], bias(O_BIN, 32 + c), GC[:],
                                                                      ALU.add, ALU.mult),
                     R=[bps_v, bGC, self.bPFM], W=[bGV])
            if tt < 3:
                self.act(lambda e, c=c: e.activation(out=self.HBv[:, c, :], in_=GV[:, 512:514], func=AF.Copy),
                         R=[bGV], W=[self.bHB[c]])
            self.dve(lambda e, c=c: e.tensor_scalar(U[:], GV[:, 0:512], PFM[:, O_CBW + c * 3:O_CBW + c * 3 + 1], None,
                                                    ALU.mult), R=[bGV, self.bPFM], W=[bU])
            for k in (1, 2):
                self.dve(lambda e, c=c, k=k: e.scalar_tensor_tensor(
                    U[:], GV[:, k:k + 512], PFM[:, O_CBW + c * 3 + k:O_CBW + c * 3 + k + 1], U[:], ALU.mult, ALU.add),
                    R=[bGV, bU, self.bPFM], W=[bU])
            self.dve(lambda e, ps_gb=ps_gb, c=c: e.scalar_tensor_tensor(YB[:, c, :], ps_gb[:], bias(O_BIN, 16 + c), U[:],
                                                                        ALU.add, ALU.mult),
                     R=[bps_gb, bU, self.bPFM], W=[bYB[c]])

            ps_p, bps_p = inproj(40 + c)
            g = c // 2
            win = 2 << g
            if tt == 0:
                self.dve(lambda e: e.memset(PIN[:, 0:15], 0.0), R=[], W=[bPIN])
            else:
                self.dve(lambda e, c=c: e.tensor_copy(PIN[:, 0:15], self.HC[:, c, 0:15]), R=[self.bHC[c]], W=[bPIN])
            self.act(lambda e, ps_p=ps_p, c=c: e.activation(out=PIN[:, 15:527], in_=ps_p[:], func=AF.Identity,
                                                            bias=bias(O_BIN, 40 + c), scale=1.0),
                     R=[bps_p, self.bPFM], W=[bPIN])
            if tt < 3:
                self.act(lambda e, c=c: e.activation(out=self.HC[:, c, 0:15], in_=PIN[:, 512:527], func=AF.Copy),
                         R=[bPIN], W=[self.bHC[c]])
            src, bsrc = PIN, bPIN
            sh = 1
            pp = 0
            while sh < win:
                dst, bdst = (S0, bS0) if pp == 0 else (S1, bS1)
                lo = 2 * sh - 1
                self.dve(lambda e, src=src, dst=dst, sh=sh, lo=lo: e.tensor_tensor(
                    dst[:, lo:527], src[:, lo:527], src[:, lo - sh:527 - sh], ALU.add), R=[bsrc], W=[bdst])
                src, bsrc = dst, bdst
                sh *= 2
                pp ^= 1
            self.dve(lambda e, src=src, c=c, win=win: e.scalar_tensor_tensor(
                PL[:, c, :], src[:, 15:527], 1.0 / win, PIN[:, 15:527], ALU.mult, ALU.subtract),
                R=[bsrc, bPIN], W=[bPL[c]])
            if tt == 0:
                n = win - 1
                self.dve(lambda e, src=src, n=n: e.tensor_tensor(XH[:, 0:n], src[:, 15:15 + n], self.RC15[:, 0:n], ALU.mult),
                         R=[bsrc, self.bRC], W=[bXH])
                self.dve(lambda e, c=c, n=n: e.tensor_tensor(PL[:, c, 0:n], XH[:, 0:n], PIN[:, 15:15 + n], ALU.subtract),
                         R=[bXH, bPIN], W=[bPL[c]])

        self.dve(lambda e: e.tensor_scalar(MEANB[:], psS1[:], 1.0 / D, None, ALU.mult), R=[bpsS1], W=[bMEANB])
        self.dve(lambda e: e.tensor_tensor(XH[:], MEANB[:], MEANB[:], ALU.mult), R=[bMEANB], W=[bXH])
        self.dve(lambda e: e.scalar_tensor_tensor(RSTDB[:], psS2[:], 1.0 / D, XH[:], ALU.mult, ALU.subtract),
                 R=[bpsS2, bXH], W=[bRSTDB])
        self.act(lambda e: e.activation(out=RSTDB[:], in_=RSTDB[:], func=AF.Sqrt, bias=self.EPSC[:, 0:1], scale=1.0),
                 R=[bRSTDB, self.bRC], W=[bRSTDB])
        self.dve(lambda e: e.reciprocal(RSTDB[:], RSTDB[:]), R=[bRSTDB], W=[bRSTDB])
        for c in range(8):
            self.dve(lambda e, c=c: e.tensor_tensor(XH[:], A[:, c, :], MEANB[:], ALU.subtract), R=[bA[c], bMEANB], W=[bXH])
            self.dve(lambda e: e.tensor_tensor(XH[:], XH[:], RSTDB[:], ALU.mult), R=[bXH, bRSTDB], W=[bXH])
            self.act(lambda e, c=c: e.activation(out=A[:, c, :], in_=XH[:], func=AF.Silu,
                                                 bias=PFM[:, O_LAB + c:O_LAB + c + 1], scale=PFM[:, O_LAG + c:O_LAG + c + 1]),
                     R=[bXH, self.bPFM], W=[bA[c]])
        self.ps_n = 8

        keep = self.scr_off_keep = 3 * 8 * 512 * 2
        self.P.phase_barrier(self.DUMMY[:, 0:1])
        for b in bA + bYB + bPL:
            self.P.scratch_live.append(b)
        self.scr_off = keep
        M, bM = self.carve("M", [8, 512], BF16, nbuf=8)
        SG, bSG = self.carve("SG", [3, 512], F32, nbuf=3)
        T1, bT1 = self.carve("T1", [512], F32)
        T2, bT2 = self.carve("T2", [512], F32)
        for d in range(8):
            Wa, bWa = self.load_ws(self.wcols(self.w_a_out[l], d * 128, 128))
            ps_a, bps_a = self.ps_next()
            self.mm(ps_a[:], [(Wa[:, k, :], A[:, k, :]) for k in range(8)], R=[bWa] + bA, bps=bps_a)
            Wb, bWb = self.load_ws(self.wcols(self.w_b_out[l], d * 128, 128))
            ps_b, bps_b = self.ps_next()
            self.mm(ps_b[:], [(Wb[:, k, :], YB[:, k, :]) for k in range(8)], R=[bWb] + bYB, bps=bps_b)
            g, dd = d // 2, d % 2
            ps_c, bps_c = self.ps_next()
            self.mm(ps_c[:], [(self.PW[:, g, cc, dd * 128:(dd + 1) * 128], PL[:, 2 * g + cc, :]) for cc in range(2)],
                    R=[self.bPW, bPL[2 * g], bPL[2 * g + 1]], bps=bps_c)
            gates = []
            for br in range(3):
                ps_x, bps_x = inproj(48 + 8 * br + d)
                self.act(lambda e, ps_x=ps_x, br=br, d=d: e.activation(out=SG[:, br, :], in_=ps_x[:], func=AF.Sigmoid,
                                                                       bias=bias(O_BIN, 48 + 8 * br + d), scale=1.0),
                         R=[bps_x, self.bPFM], W=[bSG[br]])
            self.dve(lambda e, ps_a=ps_a, d=d: e.scalar_tensor_tensor(T1[:], ps_a[:], bias(O_BAO, d), SG[:, 0, :],
                                                                      ALU.add, ALU.mult),
                     R=[bps_a, bSG[0], self.bPFM], W=[bT1])
            self.dve(lambda e, ps_b=ps_b: e.tensor_tensor(T2[:], ps_b[:], SG[:, 1, :], ALU.mult), R=[bps_b, bSG[1]], W=[bT2])
            self.dve(lambda e: e.tensor_tensor(T1[:], T1[:], T2[:], ALU.add), R=[bT1, bT2], W=[bT1])
            self.dve(lambda e, ps_c=ps_c, d=d: e.scalar_tensor_tensor(T2[:], ps_c[:], bias(O_PSC, d), SG[:, 2, :],
                                                                      ALU.mult, ALU.mult),
                     R=[bps_c, bSG[2], self.bPFM], W=[bT2])
            self.dve(lambda e, d=d: e.tensor_tensor(M[:, d, :], T1[:], T2[:], ALU.add), R=[bT1, bT2], W=[bM[d]])

        if tt == 0:
            self.load_gb(self.rows[l, 0], self.rows[l, 1])
        for half in range(2):
            Wm, bWm = self.load_wl(self.wcols(self.w_mix[l], half * 512, 512))
            for s in range(4):
                ps, bps = self.ps_next()
                self.mm(ps[:], [(M[:, k, s * 128:(s + 1) * 128], Wm[:, k, :]) for k in range(8)], R=[bWm] + bM, bps=bps)
                self.resid(4 * tt + s, half, ps[:], bps)
        self.ln_batch([4 * tt + s for s in range(4)])

    def attn_kv(self, l):
        self.new_phase()
        self.KT, self.bKT = self.carve("KT", [8, MEM], BF16)
        self.V, self.bV = self.carve("V", [2, D], BF16)
        self.attn_keep = self.scr_off
        KT, bKT, V, bV = self.KT, self.bKT, self.V, self.bV
        for f in range(8):
            W, bW = self.load_ws(self.wcols(self.w_xk[l], f * 128, 128))
            ps, bps = self.ps_next()
            self.mm(ps[:, 0:MEM], [(W[:, k, :], self.MEMT[:, k, :]) for k in range(8)], R=[bW] + self.bMEMT, bps=bps)
            self.act(lambda e, ps=ps, f=f: e.activation(out=KT[:, f, :], in_=ps[:, 0:MEM], func=AF.Copy), R=[bps], W=[bKT])
        for half in range(2):
            W, bW = self.load_wl(self.wcols(self.w_xv[l], half * 512, 512))
            for mc in range(2):
                ps, bps = self.ps_next()
                self.mm(ps[:], [(self.MEMT[:, k, mc * 128:(mc + 1) * 128], W[:, k, :]) for k in range(8)],
                        R=[bW] + self.bMEMT, bps=bps)
                self.act(lambda e, ps=ps, mc=mc, half=half: e.activation(out=V[:, mc, half * 512:(half + 1) * 512],
                                                                         in_=ps[:], func=AF.Copy), R=[bps], W=[bV])
        self.load_gb(self.rows[l, 2], self.rows[l, 3])

    def attn_tile(self, l, tt):
        P = self.P
        KT, bKT, V, bV = self.KT, self.bKT, self.V, self.bV
        xt_bufs = [self.bXT[4 * tt + s] for s in range(4)]
        tsl = slice(tt * 512, (tt + 1) * 512)
        self.P.phase_barrier(self.DUMMY[:, 0:1])
        self.P.scratch_live.extend([bKT, bV])
        self.scr_off = self.attn_keep
        QT, bQT = self.carve("QT", [8, 512], BF16, nbuf=8)
        OT, bOT = self.carve("OT", [8, 512], BF16, nbuf=8)
        PT, bPT = self.carve("PT", [2, 2, 512], BF16, nbuf=2)
        PF, bPF = self.carve("PF", [2, MEM], F32, nbuf=2)
        PN, bPN = self.carve("PN", [2, MEM], BF16, nbuf=2)
        SM, bSM = self.carve("SM", [4, 4], F32, nbuf=4)
        for f in range(8):
            W, bW = self.load_ws(self.wcols(self.w_xq[l], f * 128, 128))
            ps, bps = self.ps_next()
            self.mm(ps[:], [(W[:, k, :], self.XT[:, k, tsl]) for k in range(8)], R=[bW] + xt_bufs, bps=bps)
            self.act(lambda e, ps=ps, f=f: e.activation(out=QT[:, f, :], in_=ps[:], func=AF.Copy), R=[bps], W=[bQT[f]])
        it = 0
        for h in range(4):
            pt = h % 2
            for s in range(4):
                r = it % 2
                q = it % 4
                it += 1
                ps, bps = self.ps_next()
                self.mm(ps[:, 0:MEM], [(QT[:, 2 * h + kk, s * 128:(s + 1) * 128], KT[:, 2 * h + kk, :]) for kk in range(2)],
                        R=[bQT[2 * h], bQT[2 * h + 1], bKT], bps=bps)
                self.dve(lambda e, ps=ps, q=q: e.reduce_max(SM[:, q, 0:1], ps[:, 0:MEM], AX.X), R=[bps], W=[bSM[q]])
                self.dve(lambda e, q=q: e.tensor_scalar(SM[:, q, 1:2], SM[:, q, 0:1], -1.0 / 16.0, None, ALU.mult),
                         R=[bSM[q]], W=[bSM[q]])
                self.act(lambda e, ps=ps, r=r, q=q: e.activation(out=PF[:, r, :], in_=ps[:, 0:MEM], func=AF.Exp,
                                                                 bias=SM[:, q, 1:2], scale=1.0 / 16.0),
                         R=[bps, bSM[q]], W=[bPF[r]])
                self.dve(lambda e, r=r, q=q: e.reduce_sum(SM[:, q, 2:3], PF[:, r, :], AX.X), R=[bPF[r]], W=[bSM[q]])
                self.dve(lambda e, q=q: e.reciprocal(SM[:, q, 3:4], SM[:, q, 2:3]), R=[bSM[q]], W=[bSM[q]])
                self.dve(lambda e, r=r, q=q: e.tensor_scalar(PN[:, r, :], PF[:, r, :], SM[:, q, 3:4], None, ALU.mult),
                         R=[bPF[r], bSM[q]], W=[bPN[r]])
                ps2, bps2 = self.ps_next()
                ps2b = ps2[:].bitcast(BF16)

                def tr(e, r=r, ps2b=ps2b):
                    for mc in range(2):
                        ins = e.transpose(ps2b[:, mc * 128:(mc + 1) * 128], PN[:, r, mc * 128:(mc + 1) * 128], self.IDENT[:])
                    return ins

                P.op("pe", tr, R=[bPN[r], self.bID], W=[bps2])
                self.act(lambda e, ps2b=ps2b, pt=pt, s=s: e.activation(
                    out=PT[:, pt, :, s * 128:(s + 1) * 128], in_=ps2b[:, 0:256].rearrange("p (m t) -> p m t", m=2),
                    func=AF.Copy), R=[bps2], W=[bPT[pt]])
            for kk in range(2):
                ps, bps = self.ps_next()
                self.mm(ps[:], [(V[:, mc, (2 * h + kk) * 128:(2 * h + kk + 1) * 128], PT[:, pt, mc, :]) for mc in range(2)],
                        R=[bV, bPT[pt]], bps=bps)
                self.act(lambda e, ps=ps, h=h, kk=kk: e.activation(out=OT[:, 2 * h + kk, :], in_=ps[:], func=AF.Copy),
                         R=[bps], W=[bOT[2 * h + kk]])
        for half in range(2):
            Wo, bWo = self.load_wl(self.wcols(self.w_xo[l], half * 512, 512))
            for s in range(4):
                ps, bps = self.ps_next()
                self.mm(ps[:], [(OT[:, k, s * 128:(s + 1) * 128], Wo[:, k, :]) for k in range(8)], R=[bWo] + bOT, bps=bps)
                self.resid(4 * tt + s, half, ps[:], bps)
        self.ln_batch([4 * tt + s for s in range(4)])

    def moe(self, l):
        P = self.P
        PFM = self.PFM
        self.new_phase()
        HT, bHT = self.carve("HT", [8, T], BF16, nbuf=8)
        G2, bG2 = self.carve("G2", [16, NE], F32, nbuf=16)
        GT, bGT = self.carve("GT", [T], BF16, nbuf=16)
        LG, bLG = self.carve("LG", [2, NE], F32, nbuf=2)
        EX, bEX = self.carve("EX", [2, NE], F32, nbuf=2)
        M8, bM8 = self.carve("M8", [2, 12], F32, nbuf=2)
        GBF, bGBF = self.carve("GBF", [2, NE], BF16, nbuf=2)
        GCt, bGCt = self.carve("GCt", [2, 512], F32, nbuf=2)
        UCt, bUCt = self.carve("UCt", [2, 512], F32, nbuf=2)
        TS, bTS, RL, bRL = GCt, bGCt, UCt, bUCt
        for i in range(16):
            r = i % 2
            ps, bps = self.ps_next()
            self.mm(ps[:, 0:NE], [(self.XT[:, k, i * 128:(i + 1) * 128], self.RW[:, k, :]) for k in range(8)],
                    R=[self.bXT[i], self.bRW], bps=bps)
            self.dve(lambda e, ps=ps, r=r: e.tensor_tensor(LG[:, r, :], ps[:, 0:NE], self.RB[:], ALU.add),
                     R=[bps, self.bRB], W=[bLG[r]])
            self.dve(lambda e, r=r: e.max(M8[:, r, 0:8], LG[:, r, :]), R=[bLG[r]], W=[bM8[r]])
            self.dve(lambda e, r=r: e.tensor_scalar(M8[:, r, 8:9], M8[:, r, 0:1], -1.0, None, ALU.mult), R=[bM8[r]], W=[bM8[r]])
            self.act(lambda e, r=r: e.activation(out=EX[:, r, :], in_=LG[:, r, :], func=AF.Exp, bias=M8[:, r, 8:9], scale=1.0),
                     R=[bLG[r], bM8[r]], W=[bEX[r]])
            self.dve(lambda e, r=r: e.tensor_scalar(LG[:, r, :], LG[:, r, :], M8[:, r, 3:4], None, ALU.is_ge),
                     R=[bLG[r], bM8[r]], W=[bLG[r]])
            self.dve(lambda e, r=r: e.tensor_tensor(EX[:, r, :], EX[:, r, :], LG[:, r, :], ALU.mult), R=[bEX[r], bLG[r]], W=[bEX[r]])
            self.dve(lambda e, r=r: e.reduce_sum(M8[:, r, 9:10], EX[:, r, :], AX.X), R=[bEX[r]], W=[bM8[r]])
            self.dve(lambda e, r=r: e.reciprocal(M8[:, r, 10:11], M8[:, r, 9:10]), R=[bM8[r]], W=[bM8[r]])
            self.dve(lambda e, r=r: e.tensor_scalar(GBF[:, r, :], EX[:, r, :], M8[:, r, 10:11], None, ALU.mult),
                     R=[bEX[r], bM8[r]], W=[bGBF[r]])
            self.dve(lambda e, r=r, i=i: e.tensor_scalar(G2[:, i, :], EX[:, r, :], M8[:, r, 10:11], 1.0 / 1.702,
                                                         ALU.mult, ALU.mult), R=[bEX[r], bM8[r]], W=[bG2[i]])
            ps2, bps2 = self.ps_next()
            ps2b = ps2[:].bitcast(BF16)
            P.op("pe", lambda e, r=r, ps2b=ps2b: e.transpose(ps2b[0:NE, 0:128], GBF[:, r, :], self.IDENT[:]),
                 R=[bGBF[r], self.bID], W=[bps2])
            self.act(lambda e, ps2b=ps2b, i=i: e.activation(out=GT[0:NE, i * 128:(i + 1) * 128], in_=ps2b[0:NE, 0:128],
                                                            func=AF.Copy), R=[bps2], W=[bGT[i]])
        for i in range(16):
            for half in range(2):
                ps, bps = self.ps_next()
                self.mm(ps[:], [(GT[0:NE, i * 128:(i + 1) * 128], self.BD[:, half * 512:(half + 1) * 512])],
                        R=[bGT[i], self.bBD], bps=bps)
                self.resid(i, half, ps[:], bps)
        it = 0
        for ex in range(self.ne):
            wgu = self.w_gu[l, ex]
            for j in range(8):
                Wg, bWg = self.load_ws(self.wcols(wgu, j * 128, 128))
                Wu, bWu = self.load_ws(self.wcols(wgu, D + j * 128, 128))
                bg = PFM[:, O_BGU + ex * 16 + j:O_BGU + ex * 16 + j + 1]
                bu = PFM[:, O_BGU + ex * 16 + 8 + j:O_BGU + ex * 16 + 8 + j + 1]
                for tt in range(4):
                    r = it % 2
                    it += 1
                    tsl = slice(tt * 512, (tt + 1) * 512)
                    xt_bufs = [self.bXT[4 * tt + s] for s in range(4)]
                    psg, bpsg = self.ps_next()
                    self.mm(psg[:], [(Wg[:, k, :], self.XT[:, k, tsl]) for k in range(8)], R=[bWg] + xt_bufs, bps=bpsg)
                    psu, bpsu = self.ps_next()
                    self.mm(psu[:], [(Wu[:, k, :], self.XT[:, k, tsl]) for k in range(8)], R=[bWu] + xt_bufs, bps=bpsu)
                    self.dve(lambda e, psg=psg, bg=bg, r=r: e.tensor_scalar(GCt[:, r, :], psg[:], bg, 7.0, ALU.add, ALU.min),
                             R=[bpsg, self.bPFM], W=[bGCt[r]])
                    self.act(lambda e, r=r: e.activation(out=TS[:, r, :], in_=GCt[:, r, :], func=AF.Silu, scale=1.702),
                             R=[bGCt[r]], W=[bTS[r]])
                    self.dve(lambda e, psu=psu, bu=bu, r=r: e.tensor_scalar(UCt[:, r, :], psu[:], bu, 7.0, ALU.add, ALU.min),
                             R=[bpsu, self.bPFM], W=[bUCt[r]])
                    self.act(lambda e, r=r: e.activation(out=RL[:, r, :], in_=UCt[:, r, :], func=AF.Relu, bias=7.0, scale=1.0),
                             R=[bUCt[r]], W=[bRL[r]])
                    self.dve(lambda e, r=r, j=j, tsl=tsl: e.scalar_tensor_tensor(HT[:, j, tsl], RL[:, r, :], -6.0, TS[:, r, :],
                                                                                 ALU.add, ALU.mult),
                             R=[bRL[r], bTS[r]], W=[bHT[j]])
            for half in range(2):
                Wd, bWd = self.load_wl(self.w_down[l, ex].rearrange("(kc p) f -> p kc f", p=128)[:, :, half * 512:(half + 1) * 512])
                for i in range(16):
                    ps, bps = self.ps_next()
                    self.mm(ps[:], [(HT[:, k, i * 128:(i + 1) * 128], Wd[:, k, :]) for k in range(8)], R=[bWd] + bHT, bps=bps)
                    xs = self.X[:, i, half * 512:(half + 1) * 512]
                    self.dve(lambda e, ps=ps, xs=xs, i=i, ex=ex: e.scalar_tensor_tensor(
                        xs, ps[:], G2[:, i, ex:ex + 1], xs, ALU.mult, ALU.add),
                        R=[bps, bG2[i], self.bX[i]], W=[self.bX[i]])
        self.load_gb(self.rows[l, 4], self.rows[l, 5])
        for i0 in range(0, 16, 4):
            self.ln_batch(list(range(i0, i0 + 4)))

    def build(self):
        nc = self.nc
        with nc.Fori(0, self.NS) as s:
            self.setup_consts()
            self.load_seq(s)
            for l in range(self.L):
                self.load_layer_params(l)
                if self.do_mixer:
                    for tt in range(4):
                        self.mixer_tile(l, tt)
                if self.do_attn:
                    self.attn_kv(l)
                    for tt in range(4):
                        self.attn_tile(l, tt)
                if self.do_moe:
                    self.moe(l)
            self.store_seq(s)
            sems = self.P.emit(self.bX)
            nc.all_engine_barrier()
            nc.gpsimd.dma_reset()
            for sm in sems:
                nc.sync.sem_clear(sm)
            nc.all_engine_barrier()
        return nc


def _pack_pfm(inp, l):
    def fm(v):
        return np.ascontiguousarray(v.reshape(-1, 128).T)

    cols = [fm(inp["b_in"][l]),
            np.ascontiguousarray(inp["conv_a_w"][l].T.reshape(8, 128, 31).transpose(1, 0, 2).reshape(128, 248)),
            fm(inp["conv_a_b"][l]), fm(inp["ln_a_g"][l]), fm(inp["ln_a_b"][l]), fm(inp["b_a_out"][l]),
            np.ascontiguousarray(inp["conv_b_w"][l].T.reshape(8, 128, 3).transpose(1, 0, 2).reshape(128, 24)),
            fm(inp["pool_scale"][l]),
            np.ascontiguousarray(inp["b_gu"][l].reshape(NE, 16, 128).transpose(2, 0, 1).reshape(128, NE * 16))]
    out = np.concatenate(cols, axis=1).astype(np.float32)
    assert out.shape == (128, NPF)
    return out


def make_inmaps(inp, layers, n_cores, nseq, x_override=None):
    ls = list(layers)
    f32 = lambda a: np.ascontiguousarray(np.asarray(a, dtype=np.float32))
    shared = {
        "memln": f32(np.stack([inp["mem_ln_g"], inp["mem_ln_b"]])),
        "ident": np.eye(128, dtype=np.float32),
        "pfm": f32(np.stack([_pack_pfm(inp, l) for l in ls])),
        "rows": f32(np.stack([np.stack([inp["ln1_g"][l], inp["ln1_b"][l], inp["ln2_g"][l], inp["ln2_b"][l],
                                        inp["ln3_g"][l], inp["ln3_b"][l]]) for l in ls])),
    }
    for k in ("router_b", "w_in", "w_a_out", "w_b_out", "pool_w", "w_mix_out", "w_xq", "w_xk", "w_xv", "w_xo",
              "router_w", "w_gu", "w_down", "b_down"):
        a = inp[k]
        shared[k] = f32(a[ls[0]:ls[-1] + 1]) if ls == list(range(ls[0], ls[-1] + 1)) else f32(a[ls])
    x = inp["x"] if x_override is None else x_override
    maps = []
    for c in range(n_cores):
        m = dict(shared)
        m["x"] = f32(x[c * nseq:(c + 1) * nseq])
        m["mem"] = f32(inp["mem"][c * nseq:(c + 1) * nseq])
        maps.append(m)
    return maps


_NC_CACHE = {}


def _program(L, NS, **kw):
    key = (L, NS, tuple(sorted(kw.items())))
    if key not in _NC_CACHE:
        _NC_CACHE[key] = Builder(L, NS, **kw).build()
    return _NC_CACHE[key]


FUSED = True


def kernel(**inputs):
    inp = {k: np.asarray(v) for k, v in inputs.items()}
    if FUSED:
        nc = _program(DEPTH, NSEQ)
        maps = make_inmaps(inp, range(DEPTH), NCORES, NSEQ)
        res = run_bass_kernel_spmd(nc, maps, core_ids=list(range(NCORES)))
        return np.concatenate([r["y"] for r in res.results], axis=0).astype(np.float32)
    x = inp["x"]
    nc = _program(1, NSEQ)
    for l in range(DEPTH):
        maps = make_inmaps(inp, [l], NCORES, NSEQ, x_override=x)
        res = run_bass_kernel_spmd(nc, maps, core_ids=list(range(NCORES)))
        x = np.concatenate([r["y"] for r in res.results], axis=0).astype(np.float32)
    return x
```
